# Optimizing a Trainium2 kernel written in Bass

```python
import math
import jax
import jax.numpy as jnp
from jax import lax
import numpy as np


D_MODEL = 4096
BATCH = 2
SEQ = 8192
DEPTH = 4

GRID_W = 64
CTX_LEN = 256
HEAD_DIM = 128
MIX_WIDTH = D_MODEL
A_WIDTH = MIX_WIDTH // 4
B_WIDTH = MIX_WIDTH // 4
C_WIDTH = MIX_WIDTH - A_WIDTH - B_WIDTH
A_GROUPS = A_WIDTH // HEAD_DIM
B_HEADS = B_WIDTH // HEAD_DIM
C_HEADS = C_WIDTH // HEAD_DIM
C_KV_HEADS = C_HEADS // 4
C_KV_WIDTH = C_KV_HEADS * HEAD_DIM
IN_WIDTH = 2 * A_WIDTH + 3 * B_WIDTH + C_WIDTH + 2 * C_KV_WIDTH
CHUNK = 128
NA_MAX_ROWS = 8
NA_COLS = 16
Q_BLOCK = 128
ROPE_THETA = 10000.0
ADA_RANK = 1024
N_MOD = 6
D_FF = 2 * D_MODEL
N_EXPERTS = 8
TOP_K = 2
D_FF_EXPERT = 3 * D_MODEL // 8
EPS = 1e-6

kernel_name = 'hybrid_parallel_heads_diffusion_trunk'


def rms_norm(x, g):
    xf = x.astype(jnp.float32)
    y = xf * lax.rsqrt(jnp.mean(xf * xf, axis=-1, keepdims=True) + EPS)
    return (y * g.astype(jnp.float32)).astype(x.dtype)


def modulate(h, shift, scale):
    return h * (1 + scale) + shift


def ada_mod(cond, w_down, w_up, b_up):
    return (jax.nn.silu(cond) @ w_down) @ w_up + b_up


def split_proj(p):
    sizes = [A_WIDTH, A_WIDTH, B_WIDTH, B_WIDTH, B_WIDTH, C_WIDTH, C_KV_WIDTH, C_KV_WIDTH]
    idx = np.cumsum(sizes)[:-1].tolist()
    return jnp.split(p, idx, axis=-1)


def to_heads(t, n_heads):
    return t.reshape(t.shape[0], t.shape[1], n_heads, HEAD_DIM)


def axial_rope(n_tokens):
    t = jnp.arange(n_tokens, dtype=jnp.int32)
    pos = jnp.stack([t // GRID_W, t % GRID_W], axis=-1).astype(jnp.float32)
    n_freq = HEAD_DIM // 4
    inv = 1.0 / (ROPE_THETA ** (jnp.arange(n_freq, dtype=jnp.float32) / n_freq))
    ang = pos[:, :, None] * inv
    return jnp.cos(ang)[:, None], jnp.sin(ang)[:, None]


def apply_rope(x, cos, sin):
    lead = x.shape[:-1]
    xf = x.astype(jnp.float32).reshape(*lead, 2, 2, HEAD_DIM // 4)
    a, b = xf[..., 0, :], xf[..., 1, :]
    out = jnp.stack([a * cos - b * sin, a * sin + b * cos], axis=-2)
    return out.reshape(x.shape).astype(x.dtype)


def chunk_token_mlp(u, v, w_s, b_s, g_v):
    bn, n, _ = v.shape
    u = jax.nn.gelu(u)
    v = rms_norm(jax.nn.gelu(v), g_v)
    vc = v.reshape(bn, n // CHUNK, CHUNK, A_GROUPS, HEAD_DIM)
    mixed = jnp.einsum('gpq,bnqgc->bnpgc', w_s, vc) + b_s.T[None, None, :, :, None]
    return u * mixed.reshape(bn, n, A_WIDTH)


def attend(q, k, v):
    bn, nq, h, dh = q.shape
    kvh = k.shape[2]
    qg = q.reshape(bn, nq, kvh, h // kvh, dh)
    s = jnp.einsum('bqkgd,bskd->bkgqs', qg, k).astype(jnp.float32) * (dh ** -0.5)
    p = jax.nn.softmax(s, axis=-1).astype(v.dtype)
    o = jnp.einsum('bkgqs,bskd->bqkgd', p, v)
    return o.reshape(bn, nq, h, dh)


def blocked_attention(q, k_all, v_all):
    bn, n, h, dh = q.shape
    qb = q.reshape(bn, n // Q_BLOCK, Q_BLOCK, h, dh).transpose(1, 0, 2, 3, 4)
    ob = lax.map(lambda qi: attend(qi, k_all, v_all), qb)
    return ob.transpose(1, 0, 2, 3, 4).reshape(bn, n, h, dh)


def neighbourhood_attention(q, k, v, k_ctx, v_ctx, rpb):
    bn, n, h, dh = q.shape
    rows = n // GRID_W
    win_h = min(NA_MAX_ROWS, rows)
    n_loc = win_h * NA_COLS
    qg = q.reshape(bn, rows, GRID_W, h, dh)
    kg = k.reshape(bn, rows, GRID_W, h, dh)
    vg = v.reshape(bn, rows, GRID_W, h, dh)
    cols = np.arange(GRID_W)
    col_start = np.clip(cols - NA_COLS // 2, 0, GRID_W - NA_COLS)
    col_idx_np = col_start[:, None] + np.arange(NA_COLS)[None, :]
    col_idx = jnp.asarray(col_idx_np, dtype=jnp.int32)
    col_bias_idx = jnp.asarray(col_idx_np - cols[:, None] + (NA_COLS - 1), dtype=jnp.int32)
    scale = dh ** -0.5

    def row_block(r):
        start = jnp.clip(r - win_h // 2, 0, rows - win_h)
        q_r = lax.dynamic_index_in_dim(qg, r, axis=1, keepdims=False)
        k_band = lax.dynamic_slice_in_dim(kg, start, win_h, axis=1)
        v_band = lax.dynamic_slice_in_dim(vg, start, win_h, axis=1)
        k_n = k_band[:, :, col_idx]
        v_n = v_band[:, :, col_idx]
        row_bias_idx = start + jnp.arange(win_h, dtype=jnp.int32) - r + (NA_MAX_ROWS - 1)
        bias = rpb[:, row_bias_idx][:, :, col_bias_idx]
        s_loc = (jnp.einsum('bqhd,biqjhd->bhqij', q_r, k_n).astype(jnp.float32) * scale
                 + bias.transpose(0, 2, 1, 3)[None].astype(jnp.float32))
        s_ctx = jnp.einsum('bqhd,bchd->bhqc', q_r, k_ctx).astype(jnp.float32) * scale
        s = jnp.concatenate([s_loc.reshape(bn, h, GRID_W, n_loc), s_ctx], axis=-1)
        p = jax.nn.softmax(s, axis=-1).astype(v.dtype)
        p_loc = p[..., :n_loc].reshape(bn, h, GRID_W, win_h, NA_COLS)
        return (jnp.einsum('bhqij,biqjhd->bqhd', p_loc, v_n)
                + jnp.einsum('bhqc,bchd->bqhd', p[..., n_loc:], v_ctx))

    out = lax.map(row_block, jnp.arange(rows, dtype=jnp.int32))
    return out.transpose(1, 0, 2, 3, 4).reshape(bn, n, h * dh)


def merge_groups(y_a, y_b, y_c, g):
    g_a, g_b, g_c = jnp.split(g, [A_WIDTH, A_WIDTH + B_WIDTH])
    return jnp.concatenate([rms_norm(y_a, g_a), rms_norm(y_b, g_b), rms_norm(y_c, g_c)], axis=-1)


def token_mixers(h, hc, w_in, sgu_g, sgu_w, sgu_b, rpb, qk_g, grp_g, w_out, cos, sin, need_ctx):
    bn, n, _ = h.shape
    bc, m, _ = hc.shape
    au, av, bq, bk, bv, cq, ck, cv = split_proj(h @ w_in)
    au_c, av_c, bq_c, bk_c, bv_c, cq_c, ck_c, cv_c = split_proj(hc @ w_in)
    y_a = chunk_token_mlp(au, av, sgu_w, sgu_b, sgu_g)
    kb_c, vb_c = to_heads(bk_c, B_HEADS), to_heads(bv_c, B_HEADS)
    y_b = neighbourhood_attention(to_heads(bq, B_HEADS), to_heads(bk, B_HEADS), to_heads(bv, B_HEADS),
                                  kb_c, vb_c, rpb)
    q_c = apply_rope(rms_norm(to_heads(cq, C_HEADS), qk_g[0]), cos, sin)
    k_c = apply_rope(rms_norm(to_heads(ck, C_KV_HEADS), qk_g[1]), cos, sin)
    kc_ctx = rms_norm(to_heads(ck_c, C_KV_HEADS), qk_g[1])
    vc_ctx = to_heads(cv_c, C_KV_HEADS)
    y_c = blocked_attention(q_c, jnp.concatenate([k_c, kc_ctx], axis=1),
                            jnp.concatenate([to_heads(cv, C_KV_HEADS), vc_ctx], axis=1)).reshape(bn, n, C_WIDTH)
    y = merge_groups(y_a, y_b, y_c, grp_g) @ w_out
    if not need_ctx:
        return y, None
    z_a = chunk_token_mlp(au_c, av_c, sgu_w, sgu_b, sgu_g)
    z_b = attend(to_heads(bq_c, B_HEADS), kb_c, vb_c).reshape(bc, m, B_WIDTH)
    z_c = attend(rms_norm(to_heads(cq_c, C_HEADS), qk_g[0]), kc_ctx, vc_ctx).reshape(bc, m, C_WIDTH)
    z = merge_groups(z_a, z_b, z_c, grp_g) @ w_out
    return y, z


def swiglu(h, w1, w3, w2):
    return (jax.nn.silu(h @ w1) * (h @ w3)) @ w2


def moe_swiglu(h, w_router, b_router, w1, w3, w2):
    logits = (h @ w_router).astype(jnp.float32) + b_router.astype(jnp.float32)
    top_val, top_idx = lax.top_k(logits, TOP_K)
    gates = jax.nn.softmax(top_val, axis=-1)
    dense_gate = jnp.sum(jax.nn.one_hot(top_idx, N_EXPERTS, dtype=jnp.float32) * gates[..., None], axis=-2)
    out = jnp.zeros_like(h)
    for e in range(N_EXPERTS):
        out = out + dense_gate[..., e:e + 1].astype(h.dtype) * swiglu(h, w1[e], w3[e], w2[e])
    return out


def setup_inputs(seed: int = 0) -> dict:
    key = jax.random.key(seed)
    ks = jax.random.split(key, 26)
    f32 = jnp.float32
    n_dense = (DEPTH + 1) // 2
    n_moe = DEPTH // 2

    def nrm(k, shape, scale):
        return jax.random.normal(k, shape, f32) * scale

    return {
        'x': nrm(ks[0], (BATCH, SEQ, D_MODEL), 1.0),
        'c': nrm(ks[1], (BATCH, D_MODEL), 1.0),
        'ctx': nrm(ks[2], (BATCH, CTX_LEN, D_MODEL), 1.0),
        'c_ctx': nrm(ks[3], (D_MODEL,), 1.0),
        'ada_down': nrm(ks[4], (DEPTH, D_MODEL, ADA_RANK), D_MODEL ** -0.5),
        'ada_up': nrm(ks[5], (DEPTH, ADA_RANK, N_MOD * D_MODEL), 0.5 * ADA_RANK ** -0.5),
        'ada_bias': nrm(ks[6], (DEPTH, N_MOD * D_MODEL), 0.01),
        'norm1_g': 1.0 + nrm(ks[7], (DEPTH, D_MODEL), 0.02),
        'norm2_g': 1.0 + nrm(ks[8], (DEPTH, D_MODEL), 0.02),
        'w_in': nrm(ks[9], (DEPTH, D_MODEL, IN_WIDTH), D_MODEL ** -0.5),
        'sgu_norm_g': 1.0 + nrm(ks[10], (DEPTH, A_WIDTH), 0.02),
        'sgu_w': nrm(ks[11], (DEPTH, A_GROUPS, CHUNK, CHUNK), CHUNK ** -0.5),
        'sgu_b': 1.0 + nrm(ks[12], (DEPTH, A_GROUPS, CHUNK), 0.02),
        'na_rpb': nrm(ks[13], (DEPTH, B_HEADS, 2 * NA_MAX_ROWS - 1, 2 * NA_COLS - 1), 0.1),
        'qk_norm_g': 1.0 + nrm(ks[14], (DEPTH, 2, HEAD_DIM), 0.02),
        'group_norm_g': 1.0 + nrm(ks[15], (DEPTH, MIX_WIDTH), 0.02),
        'w_out': nrm(ks[16], (DEPTH, MIX_WIDTH, D_MODEL), MIX_WIDTH ** -0.5),
        'ffn_w1': nrm(ks[17], (n_dense, D_MODEL, D_FF), D_MODEL ** -0.5),
        'ffn_w3': nrm(ks[18], (n_dense, D_MODEL, D_FF), D_MODEL ** -0.5),
        'ffn_w2': nrm(ks[19], (n_dense, D_FF, D_MODEL), D_FF ** -0.5),
        'moe_router': nrm(ks[20], (n_moe, D_MODEL, N_EXPERTS), D_MODEL ** -0.5),
        'moe_router_b': nrm(ks[21], (n_moe, N_EXPERTS), 0.01),
        'moe_w1': nrm(ks[22], (n_moe, N_EXPERTS, D_MODEL, D_FF_EXPERT), D_MODEL ** -0.5),
        'moe_w3': nrm(ks[23], (n_moe, N_EXPERTS, D_MODEL, D_FF_EXPERT), D_MODEL ** -0.5),
        'moe_w2': nrm(ks[24], (n_moe, N_EXPERTS, D_FF_EXPERT, D_MODEL), D_FF_EXPERT ** -0.5),
        'final_norm_g': 1.0 + nrm(ks[25], (D_MODEL,), 0.02),
    }


def reference(x, c, ctx, c_ctx, ada_down, ada_up, ada_bias, norm1_g, norm2_g, w_in, sgu_norm_g, sgu_w,
              sgu_b, na_rpb, qk_norm_g, group_norm_g, w_out, ffn_w1, ffn_w3, ffn_w2, moe_router,
              moe_router_b, moe_w1, moe_w3, moe_w2, final_norm_g):
    cos, sin = axial_rope(x.shape[1])
    xc = ctx
    for l in range(DEPTH):
        need_ctx = l < DEPTH - 1
        mod = ada_mod(c, ada_down[l], ada_up[l], ada_bias[l])[:, None, :]
        mod_c = ada_mod(c_ctx, ada_down[l], ada_up[l], ada_bias[l])[None, None, :]
        sh1, sc1, g1, sh2, sc2, g2 = jnp.split(mod, N_MOD, axis=-1)
        csh1, csc1, cg1, csh2, csc2, cg2 = jnp.split(mod_c, N_MOD, axis=-1)
        h = modulate(rms_norm(x, norm1_g[l]), sh1, sc1)
        hc = modulate(rms_norm(xc, norm1_g[l]), csh1, csc1)
        y, yc = token_mixers(h, hc, w_in[l], sgu_norm_g[l], sgu_w[l], sgu_b[l], na_rpb[l], qk_norm_g[l],
                             group_norm_g[l], w_out[l], cos, sin, need_ctx)
        x = x + g1 * y
        h2 = modulate(rms_norm(x, norm2_g[l]), sh2, sc2)
        if need_ctx:
            xc = xc + cg1 * yc
            h2c = modulate(rms_norm(xc, norm2_g[l]), csh2, csc2)
        j = l // 2
        if l % 2 == 0:
            x = x + g2 * swiglu(h2, ffn_w1[j], ffn_w3[j], ffn_w2[j])
            if need_ctx:
                xc = xc + cg2 * swiglu(h2c, ffn_w1[j], ffn_w3[j], ffn_w2[j])
        else:
            x = x + g2 * moe_swiglu(h2, moe_router[j], moe_router_b[j], moe_w1[j], moe_w3[j], moe_w2[j])
            if need_ctx:
                xc = xc + cg2 * moe_swiglu(h2c, moe_router[j], moe_router_b[j], moe_w1[j], moe_w3[j], moe_w2[j])
    return rms_norm(x, final_norm_g)
```

```python
import math
from contextlib import ExitStack
import numpy as np
import concourse.bass as bass
import concourse.mybir as mybir
from concourse.bass_utils import run_bass_kernel_spmd

F32 = mybir.dt.float32
BF16 = mybir.dt.bfloat16
AF = mybir.ActivationFunctionType
ALU = mybir.AluOpType
AX = mybir.AxisListType
NEG = -30000.0
EPS = 1e-6


class Cfg:
    def __init__(s, D=4096, BATCH=2, SEQ=8192, DEPTH=4, CTX=256, RANK=1024, NB=2):
        s.D, s.BATCH, s.N, s.L, s.M, s.RANK, s.NB = D, BATCH, SEQ, DEPTH, CTX, RANK, NB
        s.GW, s.HD = 64, 128
        s.A = D // 4; s.B = D // 4; s.C = D - s.A - s.B
        s.AG = s.A // 128; s.BH = s.B // 128; s.CH = s.C // 128; s.CKV = s.CH // 4
        s.CKW = s.CKV * 128
        s.INW = 2 * s.A + 3 * s.B + s.C + 2 * s.CKW
        s.FF = 2 * D; s.NE = 8; s.FFE = 3 * D // 8
        s.KC = D // 128
        s.NT = SEQ // 128; s.MT = CTX // 128; s.TT = s.NT + s.MT
        s.T = SEQ + CTX
        s.ROWS = SEQ // s.GW
        s.R = NB + 1
        s.NDENSE = (DEPTH + 1) // 2; s.NMOE = DEPTH // 2
        o = 0
        s.o_au = o; o += s.A
        s.o_av = o; o += s.A
        s.o_bq = o; o += s.B
        s.o_bk = o; o += s.B
        s.o_bv = o; o += s.B
        s.o_cq = o; o += s.C
        s.o_ck = o; o += s.CKW
        s.o_cv = o; o += s.CKW
        assert o == s.INW


def na_tables(cfg):
    NT, ROWS = cfg.NT, cfg.ROWS
    kinds, tile_kind, tile_s0 = {}, [], []
    for t in range(NT):
        s0 = min(max(t - 4, 0), NT - 8)
        key = []
        for a in range(2):
            r = 2 * t + a
            st = min(max(r - 4, 0), ROWS - 8)
            for kr in range(16):
                ka = 2 * s0 + kr
                key.append((ka - r) if (st <= ka < st + 8) else None)
        key = tuple(key)
        if key not in kinds:
            kinds[key] = len(kinds)
        tile_kind.append(kinds[key]); tile_s0.append(s0)
    return list(kinds.keys()), tile_kind, tile_s0


def host_nab(cfg, rpb):
    kinds, _, _ = na_tables(cfg)
    L, H = rpb.shape[0], rpb.shape[1]
    cols = np.arange(64)
    cs = np.clip(cols - 8, 0, 64 - 16)
    kj = np.arange(64)
    colvalid = (kj[None, :] >= cs[:, None]) & (kj[None, :] < cs[:, None] + 16)
    dj = np.clip(kj[None, :] - cols[:, None] + 15, 0, 30)
    out = np.full((L, len(kinds), H, 128, 1024), NEG, np.float32)
    for ki, key in enumerate(kinds):
        for a in range(2):
            for kr in range(16):
                di = key[a * 16 + kr]
                if di is None:
                    continue
                blk = rpb[:, :, di + 7, :][:, :, dj]
                blk = np.where(colvalid[None, None], blk, np.float32(NEG))
                out[:, ki, :, a * 64:(a + 1) * 64, kr * 64:(kr + 1) * 64] = blk
    return out


class Tracker:
    def __init__(s, nc, stack):
        s.nc = nc
        s.E = {}
        for name, eng in (("pe", nc.tensor), ("act", nc.scalar), ("dve", nc.vector), ("pool", nc.gpsimd), ("sp", nc.sync)):
            sem = stack.enter_context(nc.semaphore("s_" + name))
            s.E[name] = dict(eng=eng, sem=sem, n=0, sigs=[], val=0, last=None, we={}, wd={})
        s.dpool = {}
        for q, n in (("sp", 20), ("pool", 12)):
            s.dpool[q] = dict(sems=[stack.enter_context(nc.semaphore(f"d_{q}{i}")) for i in range(n)], vals=[0] * n, nxt=0)
        s.res = {}
        s.attach = True

    def _wait(s, f, tok, pend=None):
        if tok is None:
            return
        F = s.E[f]
        if tok[0] == "e":
            _, e, idx = tok
            if e == f and e in ("pe", "sp", "pool"):
                return
            E = s.E[e]
            val = None
            for (i, v) in reversed(E["sigs"]):
                if i >= idx:
                    val = v
                else:
                    break
            if val is None:
                E["val"] += 1
                E["last"].then_inc(E["sem"], 1)
                E["sigs"].append((E["n"] - 1, E["val"]))
                val = E["val"]
            if F["we"].get(e, 0) < val:
                F["we"][e] = val
                if pend is not None:
                    pend[("e", e)] = (E["sem"], val)
                else:
                    F["eng"].wait_ge(E["sem"], val)
        else:
            _, q, i, val = tok
            if F["wd"].get((q, i), 0) < val:
                F["wd"][(q, i)] = val
                if pend is not None:
                    pend[("d", q, i)] = (s.dpool[q]["sems"][i], val)
                else:
                    F["eng"].wait_ge(s.dpool[q]["sems"][i], val)

    def _deps(s, f, r, w, attach=True):
        toks = []
        for k in r:
            st = s.res.get(k)
            if st:
                toks.append(st["w"])
        for k in w:
            st = s.res.get(k)
            if st:
                toks.append(st["w"]); toks.extend(st["r"].values())
        seen = set()
        pend = {}
        for t in toks:
            if t is not None and t not in seen:
                seen.add(t); s._wait(f, t, pend)
        items = list(pend.values())
        keep = None
        if attach and items:
            keep = items.pop()
        for (sem, val) in items:
            s.E[f]["eng"].wait_ge(sem, val)
        return keep

    def _record(s, tok, r, w):
        rk = (tok[0], tok[1]) if tok[0] == "e" else (tok[0], tok[1], tok[2])
        for k in r:
            st = s.res.setdefault(k, dict(w=None, r={}))
            st["r"][rk] = tok
        for k in w:
            s.res[k] = dict(w=tok, r={})

    def op(s, e, fn, r=(), w=()):
        keep = s._deps(e, r, w, attach=s.attach)
        E = s.E[e]
        ins = fn(E["eng"])
        if keep is not None:
            ins._wait_ge(keep[0], keep[1])
        E["last"] = ins
        tok = ("e", e, E["n"])
        E["n"] += 1
        s._record(tok, r, w)
        return ins

    def dma(s, q, out, in_, r=(), w=()):
        keep = s._deps(q, r, w, attach=s.attach)
        P = s.dpool[q]
        i = P["nxt"]; P["nxt"] = (i + 1) % len(P["sems"])
        Q = s.E[q]
        if P["vals"][i] > 0 and Q["wd"].get((q, i), 0) < P["vals"][i]:
            Q["wd"][(q, i)] = P["vals"][i]
            if keep is None and s.attach:
                keep = (P["sems"][i], P["vals"][i])
            else:
                Q["eng"].wait_ge(P["sems"][i], P["vals"][i])
        P["vals"][i] += 16
        ins = Q["eng"].dma_start(out=out, in_=in_)
        if keep is not None:
            ins._wait_ge(keep[0], keep[1])
        ins.then_inc(P["sems"][i], 16)
        tok = ("d", q, i, P["vals"][i])
        s._record(tok, r, w)

    def barrier(s):
        for f in s.E:
            for e in ("pe", "act", "dve"):
                if s.E[e]["n"] > 0 and e != f:
                    s._wait(f, ("e", e, s.E[e]["n"] - 1))
            for q, P in s.dpool.items():
                for i, v in enumerate(P["vals"]):
                    if v > 0:
                        s._wait(f, ("d", q, i, v))
        s.res = {}

    def final_wait(s):
        s.barrier()


def build_program(cfg, moe_flags=None, need_ctx_flag=None, final=True, xout=False):
    c = cfg
    D, KC, L, NB, R = c.D, c.KC, c.L, c.NB, c.R
    nc = bass.Bass("TRN2", target_bir_lowering=False)

    def ein(name, shape, dt=F32):
        return nc.dram_tensor(name, list(shape), dt, kind="ExternalInput").ap()

    kinds, tile_kind, tile_s0 = na_tables(c)
    NK = len(kinds)
    xin = ein("xin", [NB * c.T, D])
    cT = ein("cT", [128, KC * R])
    ada_down = ein("ada_down", [L * D, c.RANK])
    ada_up = ein("ada_up", [L * c.RANK, 6 * D])
    ada_biasT = ein("ada_biasT", [L * 128, 6 * KC])
    n1T = ein("n1T", [L * 128, KC]); n2T = ein("n2T", [L * 128, KC]); grpT = ein("grpT", [L * 128, KC])
    w_in = ein("w_in", [L * D, c.INW])
    w_out = ein("w_out", [L * D, D])
    sgu_gb = ein("sgu_gb", [L * 128, c.A])
    sgu_wT = ein("sgu_wT", [L * c.AG * 128, 128])
    sgu_bT = ein("sgu_bT", [L * 128, c.AG])
    nab = ein("nab", [L * NK * c.BH * 128, 1024])
    qkg = ein("qkg", [L * 2 * 128, 128])
    mf = moe_flags if moe_flags is not None else [(l_ % 2 == 1) for l_ in range(L)]
    nd = sum(1 for m_ in mf if not m_); nm_ = sum(1 for m_ in mf if m_)
    if nd:
        ffn_w1 = ein("ffn_w1", [nd * D, c.FF]); ffn_w3 = ein("ffn_w3", [nd * D, c.FF]); ffn_w2 = ein("ffn_w2", [nd * c.FF, D])
    if nm_:
        moe_r = ein("moe_r", [nm_ * D, c.NE]); moe_rb = ein("moe_rb", [nm_ * 128, c.NE])
        moe_w1 = ein("moe_w1", [nm_ * c.NE * D, c.FFE]); moe_w3 = ein("moe_w3", [nm_ * c.NE * D, c.FFE]); moe_w2 = ein("moe_w2", [nm_ * c.NE * c.FFE, D])
    if final:
        fin_b = ein("fin_b", [128, D])
    ident_in = ein("ident", [128, 128])
    rope = ein("rope", [c.N, 128])
    if final:
        out = nc.dram_tensor("out", [NB * c.N, D], F32, kind="ExternalOutput").ap()
    if xout:
        xo = nc.dram_tensor("xout", [NB * c.T, D], F32, kind="ExternalOutput").ap()

    xsb = [nc.dram_tensor(f"xs{b}", [c.T, D], F32).ap() for b in range(NB)]
    proj = nc.dram_tensor("proj", [c.T, c.INW], BF16).ap()
    ysc = nc.dram_tensor("ysc", [c.T, D], F32).ap()
    qbT = nc.dram_tensor("qbT", [c.BH * 128, c.T], BF16).ap()
    kbT = nc.dram_tensor("kbT", [c.BH * 128, c.T], BF16).ap()
    qcT = nc.dram_tensor("qcT", [c.CH * 128, c.T], BF16).ap()
    kcT = nc.dram_tensor("kcT", [c.CKV * 128, c.T], BF16).ap()

    G = 4
    groups = [(g * G, G, False) for g in range(c.NT // G)] + [(c.NT, c.MT, True)]
    SLAB = 32 * 512

    with ExitStack() as top:
        tr = Tracker(nc, top)
        uid = [0]

        def sb(st, name, shape, dt):
            uid[0] += 1
            return st.enter_context(nc.sbuf_tensor(f"sb{uid[0]}_{name}", list(shape), dt))
        psf = [top.enter_context(nc.psum_tensor(f"psf{i}", [128, 512], F32)) for i in range(6)]
        psb = [top.enter_context(nc.psum_tensor(f"psb{i}", [128, 1024], BF16)) for i in range(2)]
        cnt = dict(psf=0, psb=0, slab=0, ev=0)

        def next_psf():
            i = cnt["psf"] % 6; cnt["psf"] += 1
            return i

        def next_psb():
            i = cnt["psb"] % 2; cnt["psb"] += 1
            return i

        def ev_eng():
            cnt["ev"] += 1
            return "act" if cnt["ev"] % 2 else "dve"

        def copy_op(e, o, i, r, w):
            if e == "act":
                tr.op("act", lambda g: g.activation(out=o, in_=i, func=AF.Copy), r=r, w=w)
            else:
                tr.op("dve", lambda g: g.tensor_copy(o, i), r=r, w=w)

        ident = sb(top, "ident", [128, 128], BF16)
        identf = sb(top, "identf", [128, 128], F32)
        ones_f = sb(top, "ones_f", [128, 128], F32)
        modT = sb(top, "modT", [128, 6 * KC * R], F32)
        s1T = sb(top, "s1T", [128, R * KC], F32); s2T = sb(top, "s2T", [128, R * KC], F32)
        gT_g = sb(top, "gT_g", [128, KC], F32)
        n1s = sb(top, "n1s", [128, KC], F32); n2s = sb(top, "n2s", [128, KC], F32)
        cur = {}

        def alloc_lin(st, with_act=True):
            cur["slabs"] = [sb(st, f"slab{i}", [128, SLAB], BF16) for i in range(2)]
            if with_act:
                cur["actT"] = sb(st, "actT", [128, KC * G * 128], BF16)
        stat = sb(top, "stat", [128, 64], F32)
        tr.dma("pool", ident[:], ident_in[:, :], w=["ident"])
        tr.dma("sp", identf[:], ident_in[:, :], w=["identf"])
        tr.op("dve", lambda g: g.memset(ones_f[:], 1.0), w=["ones_f"])
        for b in range(NB):
            for t in range(c.TT):
                r0 = b * c.T + t * 128
                tr.dma("sp", xsb[b][t * 128:(t + 1) * 128, :], xin[r0:r0 + 128, :], w=[f"xs{b}_{t}"])

        def load_slab(Wap, kcx, ncols, names_w=()):
            i = cnt["slab"] % 2; cnt["slab"] += 1
            view = cur["slabs"][i][:, 0:kcx * ncols].rearrange("p (k n) -> p k n", n=ncols)
            src = Wap.rearrange("(k p) n -> p k n", p=128)
            step = max(1, kcx // 4)
            for k0 in range(0, kcx, step):
                k1 = min(kcx, k0 + step)
                tr.dma("pool", view[:, k0:k1, :], src[:, k0:k1, :], w=[f"slab{i}_{k0 // step}"])
            return view, [f"slab{i}_{j}" for j in range((kcx + step - 1) // step)], step

        def rstd_from_ss(col, width, nm):
            tr.op("act", lambda g: g.activation(out=stat[:, col:col + 1], in_=stat[:, col:col + 1], func=AF.Sqrt, scale=1.0 / width, bias=EPS), r=[nm], w=[nm])
            tr.op("dve", lambda g: g.reciprocal(stat[:, col:col + 1], stat[:, col:col + 1]), r=[nm], w=[nm])

        def make_actT(ph, src_tile, src_name, j, colgroups, scaleT, biasT, sc_names):
            xn = ph["xn"]; junk = xn
            for gi, (c0, c1) in enumerate(colgroups):
                nm = f"st{gi}"
                tr.op("act", lambda g: g.activation(out=junk[:, c0:c1], in_=src_tile[:, c0:c1], func=AF.Square, accum_out=stat[:, gi:gi + 1]), r=[src_name], w=["xn", nm])
                rstd_from_ss(gi, c1 - c0, nm)
                tr.op("dve", lambda g: g.tensor_single_scalar(xn[:, c0:c1], src_tile[:, c0:c1], stat[:, gi:gi + 1], ALU.mult), r=[src_name, nm], w=["xn"])
            av = cur["actT"][:, :].rearrange("p (k t) -> p k t", t=G * 128)
            for k0 in range(0, KC, 8):
                pi = next_psb()
                for kc in range(k0, min(KC, k0 + 8)):
                    tr.op("pe", lambda g: g.transpose(psb[pi][:, (kc - k0) * 128:(kc - k0 + 1) * 128], xn[:, kc * 128:(kc + 1) * 128], ident[:]), r=["xn", "ident"], w=[f"psb{pi}"])
                for kc in range(k0, min(KC, k0 + 8)):
                    src = psb[pi][:, (kc - k0) * 128:(kc - k0 + 1) * 128]
                    dst = av[:, kc, j * 128:(j + 1) * 128]
                    e = ev_eng()
                    if e == "act":
                        if biasT is None:
                            tr.op("act", lambda g: g.activation(out=dst, in_=src, func=AF.Identity, scale=scaleT[:, kc:kc + 1]), r=[f"psb{pi}"] + sc_names, w=["actT"])
                        else:
                            tr.op("act", lambda g: g.activation(out=dst, in_=src, func=AF.Identity, scale=scaleT[:, kc:kc + 1], bias=biasT[:, kc:kc + 1]), r=[f"psb{pi}"] + sc_names, w=["actT"])
                    else:
                        if biasT is None:
                            tr.op("dve", lambda g: g.tensor_single_scalar(dst, src, scaleT[:, kc:kc + 1], ALU.mult), r=[f"psb{pi}"] + sc_names, w=["actT"])
                        else:
                            tr.op("dve", lambda g: g.tensor_scalar(dst, src, scaleT[:, kc:kc + 1], biasT[:, kc:kc + 1], ALU.mult, ALU.add), r=[f"psb{pi}"] + sc_names, w=["actT"])

        def linear(Wap, K, ncols_total, nt, evac, lhs_of=None, kcx=None):
            kcx = K // 128
            av = cur["actT"][:, :].rearrange("p (k t) -> p k t", t=G * 128)
            for c0 in range(0, ncols_total, 512):
                w = min(512, ncols_total - c0)
                view, names, step = load_slab(Wap[:, c0:c0 + w], kcx, w)
                for j in range(nt):
                    pi = next_psf()
                    for kc in range(kcx):
                        lhs = lhs_of(kc, j) if lhs_of else av[:, kc, j * 128:(j + 1) * 128]
                        tr.op("pe", lambda g: g.matmul(psf[pi][:, 0:w], lhs, view[:, kc, :], start=(kc == 0), stop=(kc == kcx - 1)),
                              r=["actT", "gT", names[kc // step]], w=[f"psf{pi}"])
                    evac(j, c0, w, psf[pi][:, 0:w], f"psf{pi}")

        def linearT(Wap, K, ncols_total, ntok, evacT):
            kcx = K // 128
            av = cur["actT"][:, :].rearrange("p (k t) -> p k t", t=G * 128)
            for c0 in range(0, ncols_total, 512):
                w = min(512, ncols_total - c0)
                view, names, step = load_slab(Wap[:, c0:c0 + w], kcx, w)
                for f0 in range(0, w, 128):
                    pi = next_psf()
                    for kc in range(kcx):
                        tr.op("pe", lambda g: g.matmul(psf[pi][:, 0:ntok], view[:, kc, f0:f0 + 128], av[:, kc, 0:ntok], start=(kc == 0), stop=(kc == kcx - 1)),
                              r=["actT", names[kc // step]], w=[f"psf{pi}"])
                    evacT((c0 + f0) // 128, psf[pi][:, 0:ntok], f"psf{pi}")

        for l in range(L):
            need_ctx = need_ctx_flag if need_ctx_flag is not None else (l < L - 1)
            with ExitStack() as ph:
                alloc_lin(ph, with_act=False)
                scT = sb(ph, "scT", [128, KC * R], BF16)
                cTs = sb(ph, "cTs", [128, KC * R], F32)
                tT = sb(ph, "tT", [128, (c.RANK // 128) * R], BF16)
                abT = sb(ph, "abT", [128, 6 * KC], F32)
                tr.dma("sp", cTs[:], cT[:, :], w=["cTs"])
                tr.dma("sp", abT[:], ada_biasT[l * 128:(l + 1) * 128, :], w=["abT"])
                tr.dma("sp", n1s[:], n1T[l * 128:(l + 1) * 128, :], w=["n1s"])
                tr.dma("sp", n2s[:], n2T[l * 128:(l + 1) * 128, :], w=["n2s"])
                tr.dma("sp", gT_g[:], grpT[l * 128:(l + 1) * 128, :], w=["gT_g"])
                tr.op("act", lambda g: g.activation(out=scT[:], in_=cTs[:], func=AF.Silu), r=["cTs"], w=["scT"])
                scv = scT[:, :].rearrange("p (k r) -> p k r", r=R)
                tv = tT[:, :].rearrange("p (k r) -> p k r", r=R)
                RC = c.RANK // 128
                for c0 in range(0, c.RANK, 512):
                    w = min(512, c.RANK - c0)
                    view, names, step = load_slab(ada_down[l * D:(l + 1) * D, c0:c0 + w], KC, w)
                    for f0 in range(0, w, 128):
                        pi = next_psf()
                        for kc in range(KC):
                            tr.op("pe", lambda g: g.matmul(psf[pi][:, 0:R], view[:, kc, f0:f0 + 128], scv[:, kc, :], start=(kc == 0), stop=(kc == KC - 1)),
                                  r=["scT", names[kc // step]], w=[f"psf{pi}"])
                        rc = (c0 + f0) // 128
                        tr.op("dve", lambda g: g.tensor_copy(tv[:, rc, :], psf[pi][:, 0:R]), r=[f"psf{pi}"], w=["tT"])
                mv = modT[:, :].rearrange("p (j r) -> p j r", r=R)
                cols_per = max(512, (SLAB // RC) // 512 * 512)
                cols_per = min(cols_per, 2048)
                for c0 in range(0, 6 * D, cols_per):
                    w = min(cols_per, 6 * D - c0)
                    view, names, step = load_slab(ada_up[l * c.RANK:(l + 1) * c.RANK, c0:c0 + w], RC, w)
                    for f0 in range(0, w, 128):
                        pi = next_psf()
                        for kc in range(RC):
                            tr.op("pe", lambda g: g.matmul(psf[pi][:, 0:R], view[:, kc, f0:f0 + 128], tv[:, kc, :], start=(kc == 0), stop=(kc == RC - 1)),
                                  r=["tT", names[kc // step]], w=[f"psf{pi}"])
                        jj = (c0 + f0) // 128
                        tr.op("act", lambda g: g.activation(out=mv[:, jj, :], in_=psf[pi][:, 0:R], func=AF.Identity, bias=abT[:, jj:jj + 1]), r=[f"psf{pi}", "abT"], w=["modT"])
                for (sT, ns, m_sc, nm) in ((s1T, n1s, 1, "s1T"), (s2T, n2s, 4, "s2T")):
                    sv = sT[:, :].rearrange("p (r k) -> p r k", k=KC)
                    for r_ in range(R):
                        tr.op("dve", lambda g: g.scalar_tensor_tensor(out=sv[:, r_, :], in0=mv[:, m_sc * KC:(m_sc + 1) * KC, r_], scalar=1.0, in1=ns[:], op0=ALU.add, op1=ALU.mult),
                              r=["modT", "n1s", "n2s"], w=[nm])
                tr.barrier()

            def mod_ap(m, r_):
                return modT[:, :].rearrange("p (j r) -> p j r", r=R)[:, m * KC:(m + 1) * KC, r_]

            for b in range(NB):
                xb0 = b * c.T

                def crow(isctx):
                    return NB if isctx else b

                with ExitStack() as ph_:
                    alloc_lin(ph_)
                    ph = dict(xn=sb(ph_, "xn", [128, D], BF16))
                    xt = [sb(ph_, f"xt{i}", [128, D], F32) for i in range(2)]
                    stg = [sb(ph_, f"stg{i}", [128, 512], BF16) for i in range(4)]
                    k_ = dict(x=0, s=0)
                    for (t0, nt, isctx) in groups:
                        r_ = crow(isctx)
                        sv = s1T[:, :].rearrange("p (r k) -> p r k", k=KC)[:, r_, :]
                        for j in range(nt):
                            xi = k_["x"] % 2; k_["x"] += 1
                            rr = (t0 + j) * 128
                            tr.dma("sp", xt[xi][:], xsb[b][rr:rr + 128, :], r=[f"xs{b}_{t0 + j}"], w=[f"xt{xi}"])
                            make_actT(ph, xt[xi], f"xt{xi}", j, [(0, D)], sv, mod_ap(0, r_), ["s1T", "modT"])

                        def evac(j, c0, w, ps, psn):
                            si = k_["s"] % 4; k_["s"] += 1
                            copy_op(ev_eng(), stg[si][:, 0:w], ps, [psn], [f"stg{si}"])
                            rr = (t0 + j) * 128
                            tr.dma("sp", proj[rr:rr + 128, c0:c0 + w], stg[si][:, 0:w], r=[f"stg{si}"], w=[f"proj{t0 + j}"])
                        linear(w_in[l * D:(l + 1) * D, :], D, c.INW, nt, evac)
                    tr.barrier()

                with ExitStack() as ph_:
                    pt = [sb(ph_, f"pt{i}", [128, c.INW], BF16) for i in range(2)]
                    gx = sb(ph_, "gx", [128, 2 * c.A], F32)
                    t1 = sb(ph_, "t1", [128, 2 * c.A], F32)
                    vn = sb(ph_, "vn", [128, c.A], BF16)
                    ya = sb(ph_, "ya", [128, c.A], F32)
                    sgb = sb(ph_, "sgb", [128, c.A], F32)
                    wsT = sb(ph_, "wsT", [128, c.AG * 128], BF16)
                    bsT = sb(ph_, "bsT", [128, c.AG], F32)
                    gq = sb(ph_, "gq", [128, 128], F32); gk = sb(ph_, "gk", [128, 128], F32)
                    NH = c.CH + c.CKV
                    cx = sb(ph_, "cx", [128, NH * 128], F32)
                    cs = sb(ph_, "cs", [128, NH * 128], F32)
                    cr = sb(ph_, "cr", [128, NH * 128], F32)
                    cb = sb(ph_, "cb", [128, NH * 128], BF16)
                    qs = sb(ph_, "qs", [128, 2 * c.B], BF16)
                    rp = sb(ph_, "rp", [128, 128], F32)
                    tst = [sb(ph_, f"tst{i}", [128, 1024], BF16) for i in range(2)]
                    tr.dma("sp", sgb[:], sgu_gb[l * 128:(l + 1) * 128, :], w=["sgb"])
                    tr.dma("pool", wsT[:, :].rearrange("p (g q) -> p g q", q=128),
                           sgu_wT[l * c.AG * 128:(l + 1) * c.AG * 128, :].rearrange("(g p) q -> p g q", p=128), w=["wsT"])
                    tr.dma("sp", bsT[:], sgu_bT[l * 128:(l + 1) * 128, :], w=["bsT"])
                    tr.dma("sp", gq[:], qkg[(l * 2) * 128:(l * 2 + 1) * 128, :], w=["gq"])
                    tr.dma("sp", gk[:], qkg[(l * 2 + 1) * 128:(l * 2 + 2) * 128, :], w=["gk"])
                    k_ = dict(t=0)

                    def transposes_to(srcs, dstT, t, srcnames):
                        for h0 in range(0, len(srcs), 8):
                            hs = srcs[h0:h0 + 8]
                            pi = next_psb()
                            for i, a in enumerate(hs):
                                tr.op("pe", lambda g: g.transpose(psb[pi][:, i * 128:(i + 1) * 128], a, ident[:]), r=srcnames + ["ident"], w=[f"psb{pi}"])
                            ti = k_["t"] % 2; k_["t"] += 1
                            copy_op(ev_eng(), tst[ti][:, 0:len(hs) * 128], psb[pi][:, 0:len(hs) * 128], [f"psb{pi}"], [f"tst{ti}"])
                            tr.dma("sp", dstT[h0 * 128:(h0 + len(hs)) * 128, t * 128:(t + 1) * 128].rearrange("(h p) q -> p h q", p=128),
                                   tst[ti][:, 0:len(hs) * 128].rearrange("p (h q) -> p h q", q=128), r=[f"tst{ti}"], w=[f"T{t}"])

                    for t in range(c.TT):
                        isctx = t >= c.NT
                        pi_ = t % 2
                        P = pt[pi_]
                        pn = f"pt{pi_}"
                        tr.dma("sp", P[:], proj[t * 128:(t + 1) * 128, :], r=[f"proj{t}"], w=[pn])
                        X = P[:, c.o_au:c.o_au + 2 * c.A]
                        tr.op("dve", lambda g: g.tensor_tensor(t1[:], X, X, ALU.mult), r=[pn], w=["t1"])
                        tr.op("dve", lambda g: g.tensor_scalar(t1[:], t1[:], 0.044715, 1.0, ALU.mult, ALU.add), r=["t1"], w=["t1"])
                        tr.op("dve", lambda g: g.tensor_tensor(t1[:], t1[:], X, ALU.mult), r=["t1", pn], w=["t1"])
                        tr.op("act", lambda g: g.activation(out=t1[:], in_=t1[:], func=AF.Sigmoid, scale=1.5957691216057308), r=["t1"], w=["t1"])
                        tr.op("dve", lambda g: g.tensor_tensor(gx[:], t1[:], X, ALU.mult), r=["t1", pn], w=["gx"])
                        tr.op("act", lambda g: g.activation(out=t1[:, 0:c.A], in_=gx[:, c.A:2 * c.A], func=AF.Square, accum_out=stat[:, 8:9]), r=["gx"], w=["t1", "st8"])
                        rstd_from_ss(8, c.A, "st8")
                        tr.op("dve", lambda g: g.scalar_tensor_tensor(out=vn[:], in0=gx[:, c.A:2 * c.A], scalar=stat[:, 8:9], in1=sgb[:], op0=ALU.mult, op1=ALU.mult), r=["gx", "st8", "sgb"], w=["vn"])
                        wv = wsT[:, :].rearrange("p (g q) -> p g q", q=128)
                        banks = []
                        for g0 in range(0, c.AG, 4):
                            pi = next_psf(); banks.append(pi)
                            for gg in range(g0, min(c.AG, g0 + 4)):
                                tr.op("pe", lambda g: g.matmul(psf[pi][:, (gg - g0) * 128:(gg - g0 + 1) * 128], wv[:, gg, :], vn[:, gg * 128:(gg + 1) * 128], start=True, stop=True),
                                      r=["wsT", "vn"], w=[f"psf{pi}"])
                        for gg in range(c.AG):
                            pi = banks[gg // 4]
                            tr.op("dve", lambda g: g.scalar_tensor_tensor(out=ya[:, gg * 128:(gg + 1) * 128], in0=psf[pi][:, (gg % 4) * 128:(gg % 4 + 1) * 128], scalar=bsT[:, gg:gg + 1],
                                                                         in1=gx[:, gg * 128:(gg + 1) * 128], op0=ALU.add, op1=ALU.mult), r=[f"psf{pi}", "bsT", "gx"], w=["ya"])
                        tr.dma("sp", ysc[t * 128:(t + 1) * 128, 0:c.A], ya[:], r=["ya"], w=[f"ysc{t}a"])
                        tr.op("dve", lambda g: g.tensor_single_scalar(qs[:, 0:c.B], P[:, c.o_bq:c.o_bq + c.B], 128 ** -0.5, ALU.mult), r=[pn], w=["qs"])
                        transposes_to([qs[:, h * 128:(h + 1) * 128] for h in range(c.BH)], qbT, t, ["qs"])
                        transposes_to([P[:, c.o_bk + h * 128:c.o_bk + (h + 1) * 128] for h in range(c.BH)], kbT, t, [pn])
                        XC = P[:, c.o_cq:c.o_cq + NH * 128]
                        tr.op("dve", lambda g: g.tensor_tensor(cs[:], XC, XC, ALU.mult), r=[pn], w=["cs"])
                        tr.op("dve", lambda g: g.tensor_reduce(stat[:, 16:16 + NH], cs[:, :].rearrange("p (h d) -> p h d", d=128), AX.X, ALU.add), r=["cs"], w=["st16"])
                        tr.op("act", lambda g: g.activation(out=stat[:, 16:16 + NH], in_=stat[:, 16:16 + NH], func=AF.Sqrt, scale=1.0 / 128, bias=EPS), r=["st16"], w=["st16"])
                        tr.op("dve", lambda g: g.reciprocal(stat[:, 16:16 + NH], stat[:, 16:16 + NH]), r=["st16"], w=["st16"])
                        tr.op("dve", lambda g: g.tensor_single_scalar(stat[:, 16:16 + c.CH], stat[:, 16:16 + c.CH], 128 ** -0.5, ALU.mult), r=["st16"], w=["st16"])
                        c3 = lambda tile_: tile_[:, :].rearrange("p (h d) -> p h d", d=128)
                        tr.op("dve", lambda g: g.tensor_tensor(c3(cx), XC.rearrange("p (h d) -> p h d", d=128), stat[:, 16:16 + NH].unsqueeze(2).broadcast_to([128, NH, 128]), ALU.mult), r=[pn, "st16"], w=["cx"])
                        tr.op("dve", lambda g: g.tensor_tensor(c3(cx)[:, 0:c.CH, :], c3(cx)[:, 0:c.CH, :], gq[:, :].unsqueeze(1).broadcast_to([128, c.CH, 128]), ALU.mult), r=["cx", "gq"], w=["cx"])
                        tr.op("dve", lambda g: g.tensor_tensor(c3(cx)[:, c.CH:NH, :], c3(cx)[:, c.CH:NH, :], gk[:, :].unsqueeze(1).broadcast_to([128, c.CKV, 128]), ALU.mult), r=["cx", "gk"], w=["cx"])
                        if not isctx:
                            tr.dma("sp", rp[:], rope[t * 128:(t + 1) * 128, :], w=["rp"])
                            x5 = cx[:, :].rearrange("p (h a f) -> p h a f", a=4, f=32)
                            o5 = cr[:, :].rearrange("p (h a f) -> p h a f", a=4, f=32)
                            s5 = cs[:, :].rearrange("p (h a f) -> p h a f", a=4, f=32)
                            for ax in range(2):
                                cosb = rp[:, ax * 32:(ax + 1) * 32].unsqueeze(1).broadcast_to([128, NH, 32])
                                sinb = rp[:, 64 + ax * 32:64 + (ax + 1) * 32].unsqueeze(1).broadcast_to([128, NH, 32])
                                a_, b_ = x5[:, :, 2 * ax, :], x5[:, :, 2 * ax + 1, :]
                                tr.op("dve", lambda g: g.tensor_tensor(o5[:, :, 2 * ax, :], a_, cosb, ALU.mult), r=["cx", "rp"], w=["cr"])
                                tr.op("dve", lambda g: g.tensor_tensor(s5[:, :, 2 * ax, :], b_, sinb, ALU.mult), r=["cx", "rp"], w=["cs"])
                                tr.op("dve", lambda g: g.tensor_tensor(o5[:, :, 2 * ax, :], o5[:, :, 2 * ax, :], s5[:, :, 2 * ax, :], ALU.subtract), r=["cr", "cs"], w=["cr"])
                                tr.op("dve", lambda g: g.tensor_tensor(o5[:, :, 2 * ax + 1, :], a_, sinb, ALU.mult), r=["cx", "rp"], w=["cr"])
                                tr.op("dve", lambda g: g.tensor_tensor(s5[:, :, 2 * ax + 1, :], b_, cosb, ALU.mult), r=["cx", "rp"], w=["cs"])
                                tr.op("dve", lambda g: g.tensor_tensor(o5[:, :, 2 * ax + 1, :], o5[:, :, 2 * ax + 1, :], s5[:, :, 2 * ax + 1, :], ALU.add), r=["cr", "cs"], w=["cr"])
                            tr.op("act", lambda g: g.activation(out=cb[:], in_=cr[:], func=AF.Copy), r=["cr"], w=["cb"])
                        else:
                            tr.op("act", lambda g: g.activation(out=cb[:], in_=cx[:], func=AF.Copy), r=["cx"], w=["cb"])
                        transposes_to([cb[:, h * 128:(h + 1) * 128] for h in range(c.CH)], qcT, t, ["cb"])
                        transposes_to([cb[:, (c.CH + h) * 128:(c.CH + h + 1) * 128] for h in range(c.CKV)], kcT, t, ["cb"])
                    tr.barrier()

                with ExitStack() as ph_:
                    NKmax = c.T
                    KTs = sb(ph_, "KTs", [128, NKmax], BF16)
                    Vs = sb(ph_, "Vs", [128, c.TT * 128], BF16)
                    S = [sb(ph_, f"S{i}", [128, NKmax], F32) for i in range(2)]
                    Pm = [sb(ph_, f"Pm{i}", [128, NKmax], BF16) for i in range(2)]
                    PT = [sb(ph_, f"PT{i}", [128, 1024], BF16) for i in range(2)]
                    qt = [sb(ph_, f"qt{i}", [128, 128], BF16) for i in range(2)]
                    bia = [sb(ph_, f"bia{i}", [128, 1024], BF16) for i in range(2)]
                    yo = [sb(ph_, f"yo{i}", [128, 128], F32) for i in range(2)]
                    k_ = dict(i=0, pt=0)

                    def attn_jobs(jobs, ktname, vname):
                        def stage1(jb):
                            i = k_["i"] % 2; k_["i"] += 1
                            jb["slot"] = i
                            tr.dma("sp", qt[i][:], jb["qsrc"], r=[jb["qres"]], w=[f"qt{i}"])
                            if jb["bias"] is not None:
                                tr.dma("pool", bia[i][:], jb["bias"], w=[f"bia{i}"])
                            nk = jb["nk"]
                            pos = 0
                            for (k0, w) in jb["chunks"]:
                                pi = next_psf()
                                hasb = jb["bias"] is not None and pos < 1024
                                tr.op("pe", lambda g: g.matmul(psf[pi][:, 0:w], qt[i][:], KTs[:, k0:k0 + w], start=True, stop=not hasb), r=[f"qt{i}", ktname], w=[f"psf{pi}"])
                                if hasb:
                                    tr.op("pe", lambda g: g.matmul(psf[pi][:, 0:w], ident[:], bia[i][:, pos:pos + w], start=False, stop=True), r=[f"bia{i}", "ident"], w=[f"psf{pi}"])
                                copy_op(ev_eng(), S[i][:, pos:pos + w], psf[pi][:, 0:w], [f"psf{pi}"], [f"S{i}"])
                                pos += w
                            sc = 32 + 4 * i
                            tr.op("dve", lambda g: g.reduce_max(stat[:, sc:sc + 1], S[i][:, 0:nk], AX.X), r=[f"S{i}"], w=[f"sa{i}"])
                            tr.op("dve", lambda g: g.tensor_single_scalar(stat[:, sc:sc + 1], stat[:, sc:sc + 1], -1.0, ALU.mult), r=[f"sa{i}"], w=[f"sa{i}"])
                            tr.op("act", lambda g: g.activation(out=Pm[i][:, 0:nk], in_=S[i][:, 0:nk], func=AF.Exp, bias=stat[:, sc:sc + 1], scale=1.0, accum_out=stat[:, sc + 1:sc + 2]), r=[f"S{i}", f"sa{i}"], w=[f"Pm{i}", f"sb{i}"])
                            tr.op("dve", lambda g: g.reciprocal(stat[:, sc + 2:sc + 3], stat[:, sc + 1:sc + 2]), r=[f"sb{i}"], w=[f"sc{i}"])

                        def stage2(jb):
                            i = jb["slot"]
                            nkt = jb["nk"] // 128
                            po = next_psf()
                            for k0 in range(0, nkt, 8):
                                ks = list(range(k0, min(nkt, k0 + 8)))
                                pi = next_psb()
                                for ii, kt in enumerate(ks):
                                    tr.op("pe", lambda g: g.transpose(psb[pi][:, ii * 128:(ii + 1) * 128], Pm[i][:, kt * 128:(kt + 1) * 128], ident[:]), r=[f"Pm{i}", "ident"], w=[f"psb{pi}"])
                                ti = k_["pt"] % 2; k_["pt"] += 1
                                copy_op(ev_eng(), PT[ti][:, 0:len(ks) * 128], psb[pi][:, 0:len(ks) * 128], [f"psb{pi}"], [f"PT{ti}"])
                                for ii, kt in enumerate(ks):
                                    vt = jb["vt"][kt]
                                    tr.op("pe", lambda g: g.matmul(psf[po][:, 0:128], PT[ti][:, ii * 128:(ii + 1) * 128], Vs[:, vt * 128:(vt + 1) * 128], start=(kt == 0), stop=(kt == nkt - 1)),
                                          r=[f"PT{ti}", vname], w=[f"psf{po}"])
                            sc = 32 + 4 * i
                            tr.op("act", lambda g: g.activation(out=yo[i][:], in_=psf[po][:, 0:128], func=AF.Identity, scale=stat[:, sc + 2:sc + 3]), r=[f"psf{po}", f"sc{i}"], w=[f"yo{i}"])
                            r0, c0 = jb["dst"]
                            tr.dma("sp", ysc[r0:r0 + 128, c0:c0 + 128], yo[i][:], r=[f"yo{i}"], w=[f"ysc{r0 // 128}h{c0}"])

                        prev = None
                        for jb in jobs:
                            stage1(jb)
                            if prev is not None:
                                stage2(prev)
                            prev = jb
                        if prev is not None:
                            stage2(prev)

                    def chunks_of(segs):
                        out_ = []
                        for (k0, w) in segs:
                            for a in range(0, w, 512):
                                out_.append((k0 + a, min(512, w - a)))
                        return out_

                    qtiles = list(range(c.NT)) + (list(range(c.NT, c.TT)) if need_ctx else [])
                    for h in range(c.BH):
                        tr.dma("sp", KTs[:, :], kbT[h * 128:(h + 1) * 128, :], r=[f"T{t}" for t in range(c.TT)], w=["KTs"])
                        tr.dma("sp", Vs[:, :].rearrange("p (t d) -> p t d", d=128),
                               proj[:, c.o_bv + h * 128:c.o_bv + (h + 1) * 128].rearrange("(t p) d -> p t d", p=128), r=[f"proj{t}" for t in range(c.TT)], w=["Vs"])
                        jobs = []
                        for t in qtiles:
                            if t < c.NT:
                                s0 = tile_s0[t]
                                segs = [(s0 * 128, 1024), (c.N, c.M)]
                                vt = list(range(s0, s0 + 8)) + list(range(c.NT, c.TT))
                                kd = tile_kind[t]
                                r0 = ((l * NK + kd) * c.BH + h) * 128
                                bias = nab[r0:r0 + 128, :]
                            else:
                                segs = [(c.N, c.M)]; vt = list(range(c.NT, c.TT)); bias = None
                            jobs.append(dict(qsrc=qbT[h * 128:(h + 1) * 128, t * 128:(t + 1) * 128], qres=f"T{t}", chunks=chunks_of(segs), nk=sum(w for _, w in segs), vt=vt, bias=bias,
                                             dst=(t * 128, c.A + h * 128)))
                        attn_jobs(jobs, "KTs", "Vs")
                    for kv in range(c.CKV):
                        tr.dma("sp", KTs[:, :], kcT[kv * 128:(kv + 1) * 128, :], r=[f"T{t}" for t in range(c.TT)], w=["KTs"])
                        tr.dma("sp", Vs[:, :].rearrange("p (t d) -> p t d", d=128),
                               proj[:, c.o_cv + kv * 128:c.o_cv + (kv + 1) * 128].rearrange("(t p) d -> p t d", p=128), r=[f"proj{t}" for t in range(c.TT)], w=["Vs"])
                        jobs = []
                        for t in qtiles:
                            for hh in range(4):
                                h = kv * 4 + hh
                                if t < c.NT:
                                    segs = [(0, c.T)]; vt = list(range(c.TT))
                                else:
                                    segs = [(c.N, c.M)]; vt = list(range(c.NT, c.TT))
                                jobs.append(dict(qsrc=qcT[h * 128:(h + 1) * 128, t * 128:(t + 1) * 128], qres=f"T{t}", chunks=chunks_of(segs), nk=sum(w for _, w in segs), vt=vt, bias=None,
                                                 dst=(t * 128, c.A + c.B + h * 128)))
                        attn_jobs(jobs, "KTs", "Vs")
                    tr.barrier()

                moe = mf[l]
                j_ffn = sum(1 for m_ in mf[:l] if m_ == moe)
                if moe:
                    blocks = [(moe_w1[(j_ffn * c.NE + e) * D:(j_ffn * c.NE + e + 1) * D, :], moe_w3[(j_ffn * c.NE + e) * D:(j_ffn * c.NE + e + 1) * D, :],
                               moe_w2[(j_ffn * c.NE + e) * c.FFE:(j_ffn * c.NE + e + 1) * c.FFE, :], c.FFE, e) for e in range(c.NE)]
                else:
                    FB = min(c.FF, 1024)
                    blocks = [(ffn_w1[j_ffn * D:(j_ffn + 1) * D, f0:f0 + FB], ffn_w3[j_ffn * D:(j_ffn + 1) * D, f0:f0 + FB],
                               ffn_w2[j_ffn * c.FF + f0:j_ffn * c.FF + f0 + FB, :], FB, None) for f0 in range(0, c.FF, FB)]
                FBmax = max(bk[3] for bk in blocks)
                with ExitStack() as ph_:
                    alloc_lin(ph_)
                    xg = sb(ph_, "xg", [128, G * D], F32)
                    xsa = sb(ph_, "xsa", [128, max(D, (FBmax // 128) * G * 128)], BF16)
                    xn = xsa[:, 0:D]
                    sa = xsa
                    ph = dict(xn=xn)
                    gb = sb(ph_, "gb", [128, D], BF16)
                    dg = sb(ph_, "dg", [128, 128], F32)
                    gT = sb(ph_, "gT", [128, (FBmax // 128) * G * 128], BF16)
                    gate = sb(ph_, "gate", [128, G * 8], F32)
                    lg = sb(ph_, "lg", [128, G * 8], F32)
                    mx8 = sb(ph_, "mx8", [128, 8], F32)
                    rw = sb(ph_, "rw", [128, KC * c.NE], BF16)
                    rbb = sb(ph_, "rbb", [128, c.NE], F32)
                    tmpy = [sb(ph_, f"tmpy{i}", [128, 512], F32) for i in range(2)]
                    if moe:
                        tr.dma("pool", rw[:, :].rearrange("p (k e) -> p k e", e=c.NE), moe_r[j_ffn * D:(j_ffn + 1) * D, :].rearrange("(k p) e -> p k e", p=128), w=["rw"])
                        tr.dma("sp", rbb[:], moe_rb[j_ffn * 128:(j_ffn + 1) * 128, :], w=["rbb"])
                    xgv = xg[:, :].rearrange("p (j d) -> p j d", d=D)
                    k_ = dict(y=0)

                    def make_gate_bcast(m, r_):
                        mvv = mod_ap(m, r_)
                        for kc in range(KC):
                            tr.op("dve", lambda g: g.tensor_single_scalar(dg[:], identf[:], mvv[:, kc:kc + 1], ALU.mult), r=["identf", "modT"], w=["dg"])
                            pi = next_psf()
                            tr.op("pe", lambda g: g.matmul(psf[pi][:, 0:128], ones_f[:], dg[:], start=True, stop=True), r=["ones_f", "dg"], w=[f"psf{pi}"])
                            copy_op("act", gb[:, kc * 128:(kc + 1) * 128], psf[pi][:, 0:128], [f"psf{pi}"], ["gb"])

                    for (t0, nt, isctx) in groups:
                        if isctx and not need_ctx:
                            continue
                        r_ = crow(isctx)
                        for j in range(nt):
                            rr = (t0 + j) * 128
                            tr.dma("sp", xgv[:, j, :], ysc[rr:rr + 128, :], w=[f"xg{j}"])
                            make_actT(ph, xgv[:, j, :], f"xg{j}", j, [(0, c.A), (c.A, c.A + c.B), (c.A + c.B, D)], gT_g, None, ["gT_g"])
                            tr.dma("sp", xgv[:, j, :], xsb[b][rr:rr + 128, :], r=[f"xs{b}_{t0 + j}"], w=[f"xg{j}"])
                        make_gate_bcast(2, r_)

                        def evac_res(j, c0, w, ps, psn):
                            yi = k_["y"] % 2; k_["y"] += 1
                            tr.op("dve", lambda g: g.tensor_tensor(tmpy[yi][:, 0:w], ps, gb[:, c0:c0 + w], ALU.mult), r=[psn, "gb"], w=[f"tmpy{yi}"])
                            tr.op("dve", lambda g: g.tensor_tensor(xgv[:, j, c0:c0 + w], xgv[:, j, c0:c0 + w], tmpy[yi][:, 0:w], ALU.add), r=[f"tmpy{yi}", f"xg{j}"], w=[f"xg{j}"])
                        linear(w_out[l * D:(l + 1) * D, :], D, D, nt, evac_res)
                        sv2 = s2T[:, :].rearrange("p (r k) -> p r k", k=KC)[:, r_, :]
                        for j in range(nt):
                            make_actT(ph, xgv[:, j, :], f"xg{j}", j, [(0, D)], sv2, mod_ap(3, r_), ["s2T", "modT"])
                        make_gate_bcast(5, r_)
                        ntok = nt * 128
                        av = cur["actT"][:, :].rearrange("p (k t) -> p k t", t=G * 128)
                        if moe:
                            rwv = rw[:, :].rearrange("p (k e) -> p k e", e=c.NE)
                            for j in range(nt):
                                pi = next_psf()
                                for kc in range(KC):
                                    tr.op("pe", lambda g: g.matmul(psf[pi][:, 0:c.NE], av[:, kc, j * 128:(j + 1) * 128], rwv[:, kc, :], start=(kc == 0), stop=(kc == KC - 1)), r=["actT", "rw"], w=[f"psf{pi}"])
                                lgj = lg[:, j * 8:(j + 1) * 8]; gj = gate[:, j * 8:(j + 1) * 8]
                                tr.op("dve", lambda g: g.tensor_tensor(lgj, psf[pi][:, 0:c.NE], rbb[:], ALU.add), r=[f"psf{pi}", "rbb"], w=["lg"])
                                tr.op("dve", lambda g: g.max(mx8[:], lgj), r=["lg"], w=["mx8"])
                                tr.op("dve", lambda g: g.tensor_single_scalar(gj, lgj, mx8[:, 1:2], ALU.is_ge), r=["lg", "mx8"], w=["gate"])
                                tr.op("dve", lambda g: g.tensor_single_scalar(mx8[:, 2:3], mx8[:, 0:1], -1.0, ALU.mult), r=["mx8"], w=["mx8"])
                                tr.op("act", lambda g: g.activation(out=lgj, in_=lgj, func=AF.Exp, bias=mx8[:, 2:3], scale=1.0), r=["lg", "mx8"], w=["lg"])
                                tr.op("dve", lambda g: g.tensor_tensor(gj, gj, lgj, ALU.mult), r=["gate", "lg"], w=["gate"])
                                tr.op("dve", lambda g: g.reduce_sum(mx8[:, 3:4], gj, AX.X), r=["gate"], w=["mx8"])
                                tr.op("dve", lambda g: g.reciprocal(mx8[:, 4:5], mx8[:, 3:4]), r=["mx8"], w=["mx8"])
                                tr.op("dve", lambda g: g.tensor_single_scalar(gj, gj, mx8[:, 4:5], ALU.mult), r=["gate", "mx8"], w=["gate"])
                        for (W1, W3, W2, FBk, e) in blocks:
                            sav = sa[:, 0:(FBk // 128) * ntok].rearrange("p (f t) -> p f t", t=ntok)
                            gv = gT[:, 0:(FBk // 128) * ntok].rearrange("p (f t) -> p f t", t=ntok)

                            def ev1(fc, ps, psn):
                                tr.op("act", lambda g: g.activation(out=sav[:, fc, :], in_=ps, func=AF.Silu), r=[psn], w=["xn"])

                            def ev3(fc, ps, psn):
                                tr.op("dve", lambda g: g.tensor_tensor(gv[:, fc, :], ps, sav[:, fc, :], ALU.mult), r=[psn, "xn"], w=["gT"])
                            linearT(W1, D, FBk, ntok, ev1)
                            linearT(W3, D, FBk, ntok, ev3)

                            def evac_ffn(j, c0, w, ps, psn):
                                yi = k_["y"] % 2; k_["y"] += 1
                                if e is None:
                                    tr.op("dve", lambda g: g.tensor_tensor(tmpy[yi][:, 0:w], ps, gb[:, c0:c0 + w], ALU.mult), r=[psn, "gb"], w=[f"tmpy{yi}"])
                                else:
                                    tr.op("dve", lambda g: g.scalar_tensor_tensor(out=tmpy[yi][:, 0:w], in0=ps, scalar=gate[:, j * 8 + e:j * 8 + e + 1], in1=gb[:, c0:c0 + w], op0=ALU.mult, op1=ALU.mult),
                                          r=[psn, "gb", "gate"], w=[f"tmpy{yi}"])
                                tr.op("dve", lambda g: g.tensor_tensor(xgv[:, j, c0:c0 + w], xgv[:, j, c0:c0 + w], tmpy[yi][:, 0:w], ALU.add), r=[f"tmpy{yi}", f"xg{j}"], w=[f"xg{j}"])
                            linear(W2, FBk, D, nt, evac_ffn, lhs_of=lambda kc, j: gv[:, kc, j * 128:(j + 1) * 128])
                        for j in range(nt):
                            rr = (t0 + j) * 128
                            tr.dma("sp", xsb[b][rr:rr + 128, :], xgv[:, j, :], r=[f"xg{j}"], w=[f"xs{b}_{t0 + j}"])
                    tr.barrier()
        if xout:
            for b in range(NB):
                for t in range(c.TT):
                    r0 = b * c.T + t * 128
                    tr.dma("sp", xo[r0:r0 + 128, :], xsb[b][t * 128:(t + 1) * 128, :], w=[f"xo{b}_{t}"])
        with ExitStack() as ph_:
          if final:
            finb = sb(ph_, "finb", [128, D], F32)
            xt = [sb(ph_, f"fx{i}", [128, D], F32) for i in range(2)]
            jk = sb(ph_, "fjk", [128, D], BF16)
            tr.dma("sp", finb[:], fin_b[:, :], w=["finb"])
            for b in range(NB):
                for t in range(c.NT):
                    i = t % 2
                    tr.dma("sp", xt[i][:], xsb[b][t * 128:(t + 1) * 128, :], w=[f"fx{i}"])
                    tr.op("act", lambda g: g.activation(out=jk[:], in_=xt[i][:], func=AF.Square, accum_out=stat[:, i:i + 1]), r=[f"fx{i}"], w=["fjk", f"fs{i}"])
                    rstd_from_ss(i, D, f"fs{i}")
                    tr.op("dve", lambda g: g.scalar_tensor_tensor(out=xt[i][:], in0=xt[i][:], scalar=stat[:, i:i + 1], in1=finb[:], op0=ALU.mult, op1=ALU.mult), r=[f"fx{i}", f"fs{i}", "finb"], w=[f"fx{i}"])
                    tr.dma("sp", out[b * c.N + t * 128:b * c.N + (t + 1) * 128, :], xt[i][:], r=[f"fx{i}"], w=[f"out{b}_{t}"])
        tr.final_wait()
    return nc


finb = None


def _build(cfg):
    global finb
    return build_program(cfg)


def prep_inputs(cfg, inp, core, xin_override=None):
    c = cfg
    f = lambda a: np.ascontiguousarray(np.asarray(a, dtype=np.float32))
    L, D, KC, NB = c.L, c.D, c.KC, c.NB
    bs = list(range(core * NB, (core + 1) * NB))
    if xin_override is not None:
        xin = xin_override
    else:
        x = f(inp["x"]); ctx = f(inp["ctx"])
        xin = np.concatenate([np.concatenate([x[b], ctx[b]], 0) for b in bs], 0)
    conds = np.stack([f(inp["c"])[b] for b in bs] + [f(inp["c_ctx"])], 0)
    cT = conds.T.reshape(KC, 128, c.R).transpose(1, 0, 2).reshape(128, KC * c.R)
    pT = lambda v: f(v).reshape(L, -1, 128).transpose(0, 2, 1)

    def rs(a, n):
        a = f(a)
        return a.reshape(-1, n) if (a.size >= n and a.size % n == 0) else np.zeros((1, n), np.float32)
    rpb = f(inp["na_rpb"])
    t = np.arange(c.N)
    pos = np.stack([t // c.GW, t % c.GW], -1).astype(np.float32)
    nf = c.HD // 4
    inv = (1.0 / (10000.0 ** (np.arange(nf, dtype=np.float32) / nf))).astype(np.float32)
    ang = (pos[:, :, None] * inv).astype(np.float32)
    rope = np.concatenate([np.cos(ang).reshape(c.N, 2 * nf), np.sin(ang).reshape(c.N, 2 * nf)], 1).astype(np.float32)
    d = dict(
        xin=xin, cT=f(cT),
        ada_down=f(inp["ada_down"]).reshape(L * D, c.RANK), ada_up=f(inp["ada_up"]).reshape(L * c.RANK, 6 * D),
        ada_biasT=f(pT(inp["ada_bias"]).reshape(L * 128, 6 * KC)),
        n1T=f(pT(inp["norm1_g"]).reshape(L * 128, KC)), n2T=f(pT(inp["norm2_g"]).reshape(L * 128, KC)), grpT=f(pT(inp["group_norm_g"]).reshape(L * 128, KC)),
        w_in=f(inp["w_in"]).reshape(L * D, c.INW), w_out=f(inp["w_out"]).reshape(L * D, D),
        sgu_gb=f(np.broadcast_to(f(inp["sgu_norm_g"])[:, None, :], (L, 128, c.A)).reshape(L * 128, c.A)),
        sgu_wT=f(f(inp["sgu_w"]).transpose(0, 1, 3, 2).reshape(L * c.AG * 128, 128)),
        sgu_bT=f(f(inp["sgu_b"]).transpose(0, 2, 1).reshape(L * 128, c.AG)),
        nab=f(host_nab(c, rpb).reshape(-1, 1024)),
        qkg=f(np.broadcast_to(f(inp["qk_norm_g"])[:, :, None, :], (L, 2, 128, 128)).reshape(L * 2 * 128, 128)),
        ffn_w1=rs(inp["ffn_w1"], c.FF), ffn_w3=rs(inp["ffn_w3"], c.FF), ffn_w2=rs(inp["ffn_w2"], D),
        moe_r=rs(inp["moe_router"], c.NE),
        moe_rb=f(np.broadcast_to(rs(inp["moe_router_b"], c.NE)[:, None, :], (rs(inp["moe_router_b"], c.NE).shape[0], 128, c.NE)).reshape(-1, c.NE)),
        moe_w1=rs(inp["moe_w1"], c.FFE), moe_w3=rs(inp["moe_w3"], c.FFE), moe_w2=rs(inp["moe_w2"], D),
        fin_b=f(np.broadcast_to(f(inp["final_norm_g"])[None, :], (128, D))),
        ident=np.eye(128, dtype=np.float32), rope=rope,
    )
    return d


def run(cfg, inputs):
    ncores = cfg.BATCH // cfg.NB
    nc = _build(cfg)
    in_maps = [prep_inputs(cfg, inputs, k) for k in range(ncores)]
    res = run_bass_kernel_spmd(nc, in_maps, core_ids=list(range(ncores)))
    outs = [res.results[k]["out"].reshape(cfg.NB, cfg.N, cfg.D) for k in range(ncores)]
    return np.concatenate(outs, 0).astype(np.float32)


def build_final_norm(rows, D):
    nc = bass.Bass("TRN2", target_bir_lowering=False)
    xf = nc.dram_tensor("xf", [rows, D], F32, kind="ExternalInput").ap()
    fin_b = nc.dram_tensor("fin_b", [128, D], F32, kind="ExternalInput").ap()
    out = nc.dram_tensor("out", [rows, D], F32, kind="ExternalOutput").ap()
    with ExitStack() as top:
        tr = Tracker(nc, top)
        sbt = lambda name, shape, dt: top.enter_context(nc.sbuf_tensor(name, list(shape), dt))
        finb = sbt("finb", [128, D], F32)
        xt = [sbt(f"fx{i}", [128, D], F32) for i in range(2)]
        jk = sbt("fjk", [128, D], BF16)
        stat = sbt("fstat", [128, 8], F32)
        tr.dma("sp", finb[:], fin_b[:, :], w=["finb"])
        for t in range(rows // 128):
            i = t % 2
            tr.dma("sp", xt[i][:], xf[t * 128:(t + 1) * 128, :], w=[f"fx{i}"])
            tr.op("act", lambda g: g.activation(out=jk[:], in_=xt[i][:], func=AF.Square, accum_out=stat[:, i:i + 1]), r=[f"fx{i}"], w=["fjk", f"fs{i}"])
            tr.op("act", lambda g: g.activation(out=stat[:, i:i + 1], in_=stat[:, i:i + 1], func=AF.Sqrt, scale=1.0 / D, bias=EPS), r=[f"fs{i}"], w=[f"fs{i}"])
            tr.op("dve", lambda g: g.reciprocal(stat[:, i:i + 1], stat[:, i:i + 1]), r=[f"fs{i}"], w=[f"fs{i}"])
            tr.op("dve", lambda g: g.scalar_tensor_tensor(out=xt[i][:], in0=xt[i][:], scalar=stat[:, i:i + 1], in1=finb[:], op0=ALU.mult, op1=ALU.mult), r=[f"fx{i}", f"fs{i}", "finb"], w=[f"fx{i}"])
            tr.dma("sp", out[t * 128:(t + 1) * 128, :], xt[i][:], r=[f"fx{i}"], w=[f"out{t}"])
        tr.final_wait()
    return nc


_PER_LAYER = ("ada_down", "ada_up", "ada_bias", "norm1_g", "norm2_g", "w_in", "sgu_norm_g", "sgu_w", "sgu_b", "na_rpb", "qk_norm_g", "group_norm_g", "w_out")
_DENSE = ("ffn_w1", "ffn_w3", "ffn_w2")
_MOE = ("moe_router", "moe_router_b", "moe_w1", "moe_w3", "moe_w2")


def run_layers(cfg_full, inputs, NB=1):
    cf = cfg_full
    ncores = cf.BATCH // NB
    cfgL = Cfg(D=cf.D, BATCH=cf.BATCH, SEQ=cf.N, DEPTH=1, CTX=cf.M, RANK=cf.RANK, NB=NB)
    progs = {}
    xcur = [None] * ncores
    for l in range(cf.L):
        moe = (l % 2 == 1)
        if moe not in progs:
            progs[moe] = build_program(cfgL, moe_flags=[moe], need_ctx_flag=True, final=False, xout=True)
        inp_l = {}
        for k, v in inputs.items():
            if k in _PER_LAYER:
                inp_l[k] = np.asarray(v)[l:l + 1]
            elif k in _DENSE:
                if not moe:
                    inp_l[k] = np.asarray(v)[l // 2:l // 2 + 1]
            elif k in _MOE:
                if moe:
                    inp_l[k] = np.asarray(v)[l // 2:l // 2 + 1]
            else:
                inp_l[k] = v
        for k in (_MOE if not moe else _DENSE):
            inp_l[k] = np.zeros((1, 1), np.float32)
        in_maps = []
        for k in range(ncores):
            d = prep_inputs(cfgL, inp_l, k, xin_override=xcur[k])
            in_maps.append(d)
        res = run_bass_kernel_spmd(progs[moe], in_maps, core_ids=list(range(ncores)))
        xcur = [np.asarray(res.results[k]["xout"]) for k in range(ncores)]
        del in_maps, res
    lat = np.concatenate([xcur[k].reshape(NB, cf.T, cf.D)[:, :cf.N, :] for k in range(ncores)], 0).reshape(cf.BATCH * cf.N, cf.D)
    n8 = 8
    rows = lat.shape[0] // n8
    ncf = build_final_norm(rows, cf.D)
    finb = np.ascontiguousarray(np.broadcast_to(np.asarray(inputs["final_norm_g"], np.float32)[None, :], (128, cf.D)))
    res = run_bass_kernel_spmd(ncf, [{"xf": np.ascontiguousarray(lat[k * rows:(k + 1) * rows]), "fin_b": finb} for k in range(n8)], core_ids=list(range(n8)))
    out = np.concatenate([np.asarray(res.results[k]["out"]) for k in range(n8)], 0)
    return out.reshape(cf.BATCH, cf.N, cf.D).astype(np.float32)


def kernel(**inputs):
    cfg = Cfg(D=4096, BATCH=2, SEQ=8192, DEPTH=4, CTX=256, RANK=1024, NB=1)
    return run_layers(cfg, inputs, NB=1)
```

```python
import math
from contextlib import ExitStack
import numpy as np
import concourse.bass as bass
import concourse.mybir as mybir
from concourse.bass_utils import run_bass_kernel_spmd

F32 = mybir.dt.float32
BF16 = mybir.dt.bfloat16
AF = mybir.ActivationFunctionType
ALU = mybir.AluOpType
AX = mybir.AxisListType
NEG = -30000.0
EPS = 1e-6


class Cfg:
    def __init__(s, D=4096, BATCH=2, SEQ=8192, DEPTH=4, CTX=256, RANK=1024, NB=2):
        s.D, s.BATCH, s.N, s.L, s.M, s.RANK, s.NB = D, BATCH, SEQ, DEPTH, CTX, RANK, NB
        s.GW, s.HD = 64, 128
        s.A = D // 4; s.B = D // 4; s.C = D - s.A - s.B
        s.AG = s.A // 128; s.BH = s.B // 128; s.CH = s.C // 128; s.CKV = s.CH // 4
        s.CKW = s.CKV * 128
        s.INW = 2 * s.A + 3 * s.B + s.C + 2 * s.CKW
        s.FF = 2 * D; s.NE = 8; s.FFE = 3 * D // 8
        s.KC = D // 128
        s.NT = SEQ // 128; s.MT = CTX // 128; s.TT = s.NT + s.MT
        s.T = SEQ + CTX
        s.ROWS = SEQ // s.GW
        s.R = NB + 1
        s.NDENSE = (DEPTH + 1) // 2; s.NMOE = DEPTH // 2
        o = 0
        s.o_au = o; o += s.A
        s.o_av = o; o += s.A
        s.o_bq = o; o += s.B
        s.o_bk = o; o += s.B
        s.o_bv = o; o += s.B
        s.o_cq = o; o += s.C
        s.o_ck = o; o += s.CKW
        s.o_cv = o; o += s.CKW
        assert o == s.INW


def na_tables(cfg):
    NT, ROWS = cfg.NT, cfg.ROWS
    kinds, tile_kind, tile_s0 = {}, [], []
    for t in range(NT):
        s0 = min(max(t - 4, 0), NT - 8)
        key = []
        for a in range(2):
            r = 2 * t + a
            st = min(max(r - 4, 0), ROWS - 8)
            for kr in range(16):
                ka = 2 * s0 + kr
                key.append((ka - r) if (st <= ka < st + 8) else None)
        key = tuple(key)
        if key not in kinds:
            kinds[key] = len(kinds)
        tile_kind.append(kinds[key]); tile_s0.append(s0)
    return list(kinds.keys()), tile_kind, tile_s0


def host_nab(cfg, rpb):
    kinds, _, _ = na_tables(cfg)
    L, H = rpb.shape[0], rpb.shape[1]
    cols = np.arange(64)
    cs = np.clip(cols - 8, 0, 64 - 16)
    kj = np.arange(64)
    colvalid = (kj[None, :] >= cs[:, None]) & (kj[None, :] < cs[:, None] + 16)
    dj = np.clip(kj[None, :] - cols[:, None] + 15, 0, 30)
    out = np.full((L, len(kinds), H, 128, 1024), NEG, np.float32)
    for ki, key in enumerate(kinds):
        for a in range(2):
            for kr in range(16):
                di = key[a * 16 + kr]
                if di is None:
                    continue
                blk = rpb[:, :, di + 7, :][:, :, dj]
                blk = np.where(colvalid[None, None], blk, np.float32(NEG))
                out[:, ki, :, a * 64:(a + 1) * 64, kr * 64:(kr + 1) * 64] = blk
    return out


class Tracker:
    def __init__(s, nc, stack):
        s.nc = nc
        s.E = {}
        for name, eng in (("pe", nc.tensor), ("act", nc.scalar), ("dve", nc.vector), ("pool", nc.gpsimd), ("sp", nc.sync)):
            sem = stack.enter_context(nc.semaphore("s_" + name))
            s.E[name] = dict(eng=eng, sem=sem, n=0, sigs=[], val=0, last=None, we={}, wd={})
        s.dpool = {}
        for q, n in (("sp", 20), ("pool", 12)):
            s.dpool[q] = dict(sems=[stack.enter_context(nc.semaphore(f"d_{q}{i}")) for i in range(n)], vals=[0] * n, nxt=0)
        s.res = {}
        s.attach = True

    def _wait(s, f, tok, pend=None):
        if tok is None:
            return
        F = s.E[f]
        if tok[0] == "e":
            _, e, idx = tok
            if e == f and e in ("pe", "sp", "pool"):
                return
            E = s.E[e]
            val = None
            for (i, v) in reversed(E["sigs"]):
                if i >= idx:
                    val = v
                else:
                    break
            if val is None:
                E["val"] += 1
                E["last"].then_inc(E["sem"], 1)
                E["sigs"].append((E["n"] - 1, E["val"]))
                val = E["val"]
            if F["we"].get(e, 0) < val:
                F["we"][e] = val
                if pend is not None:
                    pend[("e", e)] = (E["sem"], val)
                else:
                    F["eng"].wait_ge(E["sem"], val)
        else:
            _, q, i, val = tok
            if F["wd"].get((q, i), 0) < val:
                F["wd"][(q, i)] = val
                if pend is not None:
                    pend[("d", q, i)] = (s.dpool[q]["sems"][i], val)
                else:
                    F["eng"].wait_ge(s.dpool[q]["sems"][i], val)

    def _deps(s, f, r, w, attach=True):
        toks = []
        for k in r:
            st = s.res.get(k)
            if st:
                toks.append(st["w"])
        for k in w:
            st = s.res.get(k)
            if st:
                toks.append(st["w"]); toks.extend(st["r"].values())
        seen = set()
        pend = {}
        for t in toks:
            if t is not None and t not in seen:
                seen.add(t); s._wait(f, t, pend)
        items = list(pend.values())
        keep = None
        if attach and items:
            keep = items.pop()
        for (sem, val) in items:
            s.E[f]["eng"].wait_ge(sem, val)
        return keep

    def _record(s, tok, r, w):
        rk = (tok[0], tok[1]) if tok[0] == "e" else (tok[0], tok[1], tok[2])
        for k in r:
            st = s.res.setdefault(k, dict(w=None, r={}))
            st["r"][rk] = tok
        for k in w:
            s.res[k] = dict(w=tok, r={})

    def op(s, e, fn, r=(), w=()):
        keep = s._deps(e, r, w, attach=s.attach)
        E = s.E[e]
        ins = fn(E["eng"])
        if keep is not None:
            ins._wait_ge(keep[0], keep[1])
        E["last"] = ins
        tok = ("e", e, E["n"])
        E["n"] += 1
        s._record(tok, r, w)
        return ins

    def dma(s, q, out, in_, r=(), w=()):
        keep = s._deps(q, r, w, attach=s.attach)
        P = s.dpool[q]
        i = P["nxt"]; P["nxt"] = (i + 1) % len(P["sems"])
        Q = s.E[q]
        if P["vals"][i] > 0 and Q["wd"].get((q, i), 0) < P["vals"][i]:
            Q["wd"][(q, i)] = P["vals"][i]
            if keep is None and s.attach:
                keep = (P["sems"][i], P["vals"][i])
            else:
                Q["eng"].wait_ge(P["sems"][i], P["vals"][i])
        P["vals"][i] += 16
        ins = Q["eng"].dma_start(out=out, in_=in_)
        if keep is not None:
            ins._wait_ge(keep[0], keep[1])
        ins.then_inc(P["sems"][i], 16)
        tok = ("d", q, i, P["vals"][i])
        s._record(tok, r, w)

    def barrier(s):
        for f in s.E:
            for e in ("pe", "act", "dve"):
                if s.E[e]["n"] > 0 and e != f:
                    s._wait(f, ("e", e, s.E[e]["n"] - 1))
            for q, P in s.dpool.items():
                for i, v in enumerate(P["vals"]):
                    if v > 0:
                        s._wait(f, ("d", q, i, v))
        s.res = {}

    def final_wait(s):
        s.barrier()


def build_program(cfg, moe_flags=None, need_ctx_flag=None, final=True, xout=False):
    c = cfg
    D, KC, L, NB, R = c.D, c.KC, c.L, c.NB, c.R
    nc = bass.Bass("TRN2", target_bir_lowering=False)

    def ein(name, shape, dt=F32):
        return nc.dram_tensor(name, list(shape), dt, kind="ExternalInput").ap()

    kinds, tile_kind, tile_s0 = na_tables(c)
    NK = len(kinds)
    xin = ein("xin", [NB * c.T, D])
    cT = ein("cT", [128, KC * R])
    ada_down = ein("ada_down", [L * D, c.RANK])
    ada_up = ein("ada_up", [L * c.RANK, 6 * D])
    ada_biasT = ein("ada_biasT", [L * 128, 6 * KC])
    n1T = ein("n1T", [L * 128, KC]); n2T = ein("n2T", [L * 128, KC]); grpT = ein("grpT", [L * 128, KC])
    w_in = ein("w_in", [L * D, c.INW])
    w_out = ein("w_out", [L * D, D])
    sgu_gb = ein("sgu_gb", [L * 128, c.A])
    sgu_wT = ein("sgu_wT", [L * c.AG * 128, 128])
    sgu_bT = ein("sgu_bT", [L * 128, c.AG])
    nab = ein("nab", [L * NK * c.BH * 128, 1024])
    qkg = ein("qkg", [L * 2 * 128, 128])
    mf = moe_flags if moe_flags is not None else [(l_ % 2 == 1) for l_ in range(L)]
    nd = sum(1 for m_ in mf if not m_); nm_ = sum(1 for m_ in mf if m_)
    if nd:
        ffn_w1 = ein("ffn_w1", [nd * D, c.FF]); ffn_w3 = ein("ffn_w3", [nd * D, c.FF]); ffn_w2 = ein("ffn_w2", [nd * c.FF, D])
    if nm_:
        moe_r = ein("moe_r", [nm_ * D, c.NE]); moe_rb = ein("moe_rb", [nm_ * 128, c.NE])
        moe_w1 = ein("moe_w1", [nm_ * c.NE * D, c.FFE]); moe_w3 = ein("moe_w3", [nm_ * c.NE * D, c.FFE]); moe_w2 = ein("moe_w2", [nm_ * c.NE * c.FFE, D])
    if final:
        fin_b = ein("fin_b", [128, D])
    ident_in = ein("ident", [128, 128])
    rope = ein("rope", [c.N, 128])
    if final:
        out = nc.dram_tensor("out", [NB * c.N, D], F32, kind="ExternalOutput").ap()
    if xout:
        xo = nc.dram_tensor("xout", [NB * c.T, D], F32, kind="ExternalOutput").ap()

    xsb = [nc.dram_tensor(f"xs{b}", [c.T, D], F32).ap() for b in range(NB)]
    proj = nc.dram_tensor("proj", [c.T, c.INW], BF16).ap()
    ysc = nc.dram_tensor("ysc", [c.T, D], F32).ap()
    qbT = nc.dram_tensor("qbT", [c.BH * 128, c.T], BF16).ap()
    kbT = nc.dram_tensor("kbT", [c.BH * 128, c.T], BF16).ap()
    qcT = nc.dram_tensor("qcT", [c.CH * 128, c.T], BF16).ap()
    kcT = nc.dram_tensor("kcT", [c.CKV * 128, c.T], BF16).ap()

    G = 4
    groups = [(g * G, G, False) for g in range(c.NT // G)] + [(c.NT, c.MT, True)]
    SLAB = 32 * 512

    with ExitStack() as top:
        tr = Tracker(nc, top)
        uid = [0]

        def sb(st, name, shape, dt):
            uid[0] += 1
            return st.enter_context(nc.sbuf_tensor(f"sb{uid[0]}_{name}", list(shape), dt))
        psf = [top.enter_context(nc.psum_tensor(f"psf{i}", [128, 512], F32)) for i in range(6)]
        psb = [top.enter_context(nc.psum_tensor(f"psb{i}", [128, 1024], BF16)) for i in range(2)]
        cnt = dict(psf=0, psb=0, slab=0, ev=0)

        def next_psf():
            i = cnt["psf"] % 6; cnt["psf"] += 1
            return i

        def next_psb():
            i = cnt["psb"] % 2; cnt["psb"] += 1
            return i

        def ev_eng():
            cnt["ev"] += 1
            return "act" if cnt["ev"] % 2 else "dve"

        def copy_op(e, o, i, r, w):
            if e == "act":
                tr.op("act", lambda g: g.activation(out=o, in_=i, func=AF.Copy), r=r, w=w)
            else:
                tr.op("dve", lambda g: g.tensor_copy(o, i), r=r, w=w)

        ident = sb(top, "ident", [128, 128], BF16)
        identf = sb(top, "identf", [128, 128], F32)
        ones_f = sb(top, "ones_f", [128, 128], F32)
        modT = sb(top, "modT", [128, 6 * KC * R], F32)
        s1T = sb(top, "s1T", [128, R * KC], F32); s2T = sb(top, "s2T", [128, R * KC], F32)
        gT_g = sb(top, "gT_g", [128, KC], F32)
        n1s = sb(top, "n1s", [128, KC], F32); n2s = sb(top, "n2s", [128, KC], F32)
        cur = {}

        def alloc_lin(st, with_act=True):
            cur["slabs"] = [sb(st, f"slab{i}", [128, SLAB], BF16) for i in range(2)]
            if with_act:
                cur["actT"] = sb(st, "actT", [128, KC * G * 128], BF16)
        stat = sb(top, "stat", [128, 64], F32)
        tr.dma("pool", ident[:], ident_in[:, :], w=["ident"])
        tr.dma("sp", identf[:], ident_in[:, :], w=["identf"])
        tr.op("dve", lambda g: g.memset(ones_f[:], 1.0), w=["ones_f"])
        for b in range(NB):
            for t in range(c.TT):
                r0 = b * c.T + t * 128
                tr.dma("sp", xsb[b][t * 128:(t + 1) * 128, :], xin[r0:r0 + 128, :], w=[f"xs{b}_{t}"])

        wsc = {}
        SN = [[f"slab{i}_{j}" for j in range(4)] for i in range(2)]

        def wcast(name, Wap, K, Ncols):
            if name not in wsc:
                wsc[name] = nc.dram_tensor("wsc_" + name, [K, Ncols], BF16).ap()
            dst = wsc[name]
            kcx = K // 128
            for c0 in range(0, Ncols, 8192):
                w = min(8192, Ncols - c0)
                kk = max(1, SLAB // w)
                for k0 in range(0, kcx, kk):
                    k1 = min(kcx, k0 + kk)
                    i = cnt["slab"] % 2; cnt["slab"] += 1
                    view = cur["slabs"][i][:, 0:(k1 - k0) * w].rearrange("p (k n) -> p k n", n=w)
                    tr.dma("pool", view, Wap[k0 * 128:k1 * 128, c0:c0 + w].rearrange("(k p) n -> p k n", p=128), w=SN[i])
                    tr.dma("sp", dst[k0 * 128:k1 * 128, c0:c0 + w].rearrange("(k p) n -> p k n", p=128), view, r=SN[i], w=["wsc_" + name])
            return dst

        def load_slab(Wap, kcx, ncols, names_w=()):
            i = cnt["slab"] % 2; cnt["slab"] += 1
            view = cur["slabs"][i][:, 0:kcx * ncols].rearrange("p (k n) -> p k n", n=ncols)
            src = Wap.rearrange("(k p) n -> p k n", p=128)
            q = "sp" if Wap.dtype == BF16 else "pool"
            step = max(1, (kcx + 3) // 4)
            for k0 in range(0, kcx, step):
                k1 = min(kcx, k0 + step)
                tr.dma(q, view[:, k0:k1, :], src[:, k0:k1, :], w=[f"slab{i}_{k0 // step}"])
            return view, [f"slab{i}_{j}" for j in range((kcx + step - 1) // step)], step

        def rstd_from_ss(col, width, nm):
            tr.op("act", lambda g: g.activation(out=stat[:, col:col + 1], in_=stat[:, col:col + 1], func=AF.Sqrt, scale=1.0 / width, bias=EPS), r=[nm], w=[nm])
            tr.op("dve", lambda g: g.reciprocal(stat[:, col:col + 1], stat[:, col:col + 1]), r=[nm], w=[nm])

        def make_actT(ph, src_tile, src_name, j, colgroups, scaleT, biasT, sc_names):
            xn = ph["xn"]; junk = xn
            for gi, (c0, c1) in enumerate(colgroups):
                nm = f"st{gi}"
                tr.op("act", lambda g: g.activation(out=junk[:, c0:c1], in_=src_tile[:, c0:c1], func=AF.Square, accum_out=stat[:, gi:gi + 1]), r=[src_name], w=["xn", nm])
                rstd_from_ss(gi, c1 - c0, nm)
                tr.op("dve", lambda g: g.tensor_single_scalar(xn[:, c0:c1], src_tile[:, c0:c1], stat[:, gi:gi + 1], ALU.mult), r=[src_name, nm], w=["xn"])
            av = cur["actT"][:, :].rearrange("p (k t) -> p k t", t=G * 128)
            for k0 in range(0, KC, 8):
                pi = next_psb()
                for kc in range(k0, min(KC, k0 + 8)):
                    tr.op("pe", lambda g: g.transpose(psb[pi][:, (kc - k0) * 128:(kc - k0 + 1) * 128], xn[:, kc * 128:(kc + 1) * 128], ident[:]), r=["xn", "ident"], w=[f"psb{pi}"])
                for kc in range(k0, min(KC, k0 + 8)):
                    src = psb[pi][:, (kc - k0) * 128:(kc - k0 + 1) * 128]
                    dst = av[:, kc, j * 128:(j + 1) * 128]
                    e = ev_eng()
                    if e == "act":
                        if biasT is None:
                            tr.op("act", lambda g: g.activation(out=dst, in_=src, func=AF.Identity, scale=scaleT[:, kc:kc + 1]), r=[f"psb{pi}"] + sc_names, w=["actT"])
                        else:
                            tr.op("act", lambda g: g.activation(out=dst, in_=src, func=AF.Identity, scale=scaleT[:, kc:kc + 1], bias=biasT[:, kc:kc + 1]), r=[f"psb{pi}"] + sc_names, w=["actT"])
                    else:
                        if biasT is None:
                            tr.op("dve", lambda g: g.tensor_single_scalar(dst, src, scaleT[:, kc:kc + 1], ALU.mult), r=[f"psb{pi}"] + sc_names, w=["actT"])
                        else:
                            tr.op("dve", lambda g: g.tensor_scalar(dst, src, scaleT[:, kc:kc + 1], biasT[:, kc:kc + 1], ALU.mult, ALU.add), r=[f"psb{pi}"] + sc_names, w=["actT"])

        def linear(Wap, K, ncols_total, nt, evac, lhs_of=None, kcx=None):
            kcx = K // 128
            av = cur["actT"][:, :].rearrange("p (k t) -> p k t", t=G * 128)
            for c0 in range(0, ncols_total, 512):
                w = min(512, ncols_total - c0)
                view, names, step = load_slab(Wap[:, c0:c0 + w], kcx, w)
                for j in range(nt):
                    pi = next_psf()
                    for kc in range(kcx):
                        lhs = lhs_of(kc, j) if lhs_of else av[:, kc, j * 128:(j + 1) * 128]
                        tr.op("pe", lambda g: g.matmul(psf[pi][:, 0:w], lhs, view[:, kc, :], start=(kc == 0), stop=(kc == kcx - 1)),
                              r=["actT", "gT", names[kc // step]], w=[f"psf{pi}"])
                    evac(j, c0, w, psf[pi][:, 0:w], f"psf{pi}")

        def linearT(Wap, K, ncols_total, ntok, evacT):
            kcx = K // 128
            av = cur["actT"][:, :].rearrange("p (k t) -> p k t", t=G * 128)
            for c0 in range(0, ncols_total, 512):
                w = min(512, ncols_total - c0)
                view, names, step = load_slab(Wap[:, c0:c0 + w], kcx, w)
                for f0 in range(0, w, 128):
                    pi = next_psf()
                    for kc in range(kcx):
                        tr.op("pe", lambda g: g.matmul(psf[pi][:, 0:ntok], view[:, kc, f0:f0 + 128], av[:, kc, 0:ntok], start=(kc == 0), stop=(kc == kcx - 1)),
                              r=["actT", names[kc // step]], w=[f"psf{pi}"])
                    evacT((c0 + f0) // 128, psf[pi][:, 0:ntok], f"psf{pi}")

        for l in range(L):
            need_ctx = need_ctx_flag if need_ctx_flag is not None else (l < L - 1)
            moe_l = mf[l]
            jf_l = sum(1 for m_ in mf[:l] if m_ == moe_l)
            with ExitStack() as ph:
                alloc_lin(ph, with_act=False)
                W = {}
                W["ada_down"] = wcast("ada_down", ada_down[l * D:(l + 1) * D, :], D, c.RANK)
                W["ada_up"] = wcast("ada_up", ada_up[l * c.RANK:(l + 1) * c.RANK, :], c.RANK, 6 * D)
                W["w_in"] = wcast("w_in", w_in[l * D:(l + 1) * D, :], D, c.INW)
                W["w_out"] = wcast("w_out", w_out[l * D:(l + 1) * D, :], D, D)
                if moe_l:
                    W["m1"] = wcast("m1", moe_w1[jf_l * c.NE * D:(jf_l + 1) * c.NE * D, :], c.NE * D, c.FFE)
                    W["m3"] = wcast("m3", moe_w3[jf_l * c.NE * D:(jf_l + 1) * c.NE * D, :], c.NE * D, c.FFE)
                    W["m2"] = wcast("m2", moe_w2[jf_l * c.NE * c.FFE:(jf_l + 1) * c.NE * c.FFE, :], c.NE * c.FFE, D)
                else:
                    W["f1"] = wcast("f1", ffn_w1[jf_l * D:(jf_l + 1) * D, :], D, c.FF)
                    W["f3"] = wcast("f3", ffn_w3[jf_l * D:(jf_l + 1) * D, :], D, c.FF)
                    W["f2"] = wcast("f2", ffn_w2[jf_l * c.FF:(jf_l + 1) * c.FF, :], c.FF, D)
                tr.barrier()
            with ExitStack() as ph:
                alloc_lin(ph, with_act=False)
                scT = sb(ph, "scT", [128, KC * R], BF16)
                cTs = sb(ph, "cTs", [128, KC * R], F32)
                tT = sb(ph, "tT", [128, (c.RANK // 128) * R], BF16)
                abT = sb(ph, "abT", [128, 6 * KC], F32)
                tr.dma("sp", cTs[:], cT[:, :], w=["cTs"])
                tr.dma("sp", abT[:], ada_biasT[l * 128:(l + 1) * 128, :], w=["abT"])
                tr.dma("sp", n1s[:], n1T[l * 128:(l + 1) * 128, :], w=["n1s"])
                tr.dma("sp", n2s[:], n2T[l * 128:(l + 1) * 128, :], w=["n2s"])
                tr.dma("sp", gT_g[:], grpT[l * 128:(l + 1) * 128, :], w=["gT_g"])
                tr.op("act", lambda g: g.activation(out=scT[:], in_=cTs[:], func=AF.Silu), r=["cTs"], w=["scT"])
                scv = scT[:, :].rearrange("p (k r) -> p k r", r=R)
                tv = tT[:, :].rearrange("p (k r) -> p k r", r=R)
                RC = c.RANK // 128
                for c0 in range(0, c.RANK, 512):
                    w = min(512, c.RANK - c0)
                    view, names, step = load_slab(W["ada_down"][:, c0:c0 + w], KC, w)
                    for f0 in range(0, w, 128):
                        pi = next_psf()
                        for kc in range(KC):
                            tr.op("pe", lambda g: g.matmul(psf[pi][:, 0:R], view[:, kc, f0:f0 + 128], scv[:, kc, :], start=(kc == 0), stop=(kc == KC - 1)),
                                  r=["scT", names[kc // step]], w=[f"psf{pi}"])
                        rc = (c0 + f0) // 128
                        tr.op("dve", lambda g: g.tensor_copy(tv[:, rc, :], psf[pi][:, 0:R]), r=[f"psf{pi}"], w=["tT"])
                mv = modT[:, :].rearrange("p (j r) -> p j r", r=R)
                cols_per = max(512, (SLAB // RC) // 512 * 512)
                cols_per = min(cols_per, 2048)
                for c0 in range(0, 6 * D, cols_per):
                    w = min(cols_per, 6 * D - c0)
                    view, names, step = load_slab(W["ada_up"][:, c0:c0 + w], RC, w)
                    for f0 in range(0, w, 128):
                        pi = next_psf()
                        for kc in range(RC):
                            tr.op("pe", lambda g: g.matmul(psf[pi][:, 0:R], view[:, kc, f0:f0 + 128], tv[:, kc, :], start=(kc == 0), stop=(kc == RC - 1)),
                                  r=["tT", names[kc // step]], w=[f"psf{pi}"])
                        jj = (c0 + f0) // 128
                        tr.op("act", lambda g: g.activation(out=mv[:, jj, :], in_=psf[pi][:, 0:R], func=AF.Identity, bias=abT[:, jj:jj + 1]), r=[f"psf{pi}", "abT"], w=["modT"])
                for (sT, ns, m_sc, nm) in ((s1T, n1s, 1, "s1T"), (s2T, n2s, 4, "s2T")):
                    sv = sT[:, :].rearrange("p (r k) -> p r k", k=KC)
                    for r_ in range(R):
                        tr.op("dve", lambda g: g.scalar_tensor_tensor(out=sv[:, r_, :], in0=mv[:, m_sc * KC:(m_sc + 1) * KC, r_], scalar=1.0, in1=ns[:], op0=ALU.add, op1=ALU.mult),
                              r=["modT", "n1s", "n2s"], w=[nm])
                tr.barrier()

            def mod_ap(m, r_):
                return modT[:, :].rearrange("p (j r) -> p j r", r=R)[:, m * KC:(m + 1) * KC, r_]

            for b in range(NB):
                xb0 = b * c.T

                def crow(isctx):
                    return NB if isctx else b

                with ExitStack() as ph_:
                    alloc_lin(ph_)
                    ph = dict(xn=sb(ph_, "xn", [128, D], BF16))
                    xt = [sb(ph_, f"xt{i}", [128, D], F32) for i in range(2)]
                    stg = [sb(ph_, f"stg{i}", [128, 512], BF16) for i in range(4)]
                    k_ = dict(x=0, s=0)
                    for (t0, nt, isctx) in groups:
                        r_ = crow(isctx)
                        sv = s1T[:, :].rearrange("p (r k) -> p r k", k=KC)[:, r_, :]
                        for j in range(nt):
                            xi = k_["x"] % 2; k_["x"] += 1
                            rr = (t0 + j) * 128
                            tr.dma("sp", xt[xi][:], xsb[b][rr:rr + 128, :], r=[f"xs{b}_{t0 + j}"], w=[f"xt{xi}"])
                            make_actT(ph, xt[xi], f"xt{xi}", j, [(0, D)], sv, mod_ap(0, r_), ["s1T", "modT"])

                        def evac(j, c0, w, ps, psn):
                            si = k_["s"] % 4; k_["s"] += 1
                            copy_op(ev_eng(), stg[si][:, 0:w], ps, [psn], [f"stg{si}"])
                            rr = (t0 + j) * 128
                            tr.dma("sp", proj[rr:rr + 128, c0:c0 + w], stg[si][:, 0:w], r=[f"stg{si}"], w=[f"proj{t0 + j}"])
                        linear(W["w_in"], D, c.INW, nt, evac)
                    tr.barrier()

                with ExitStack() as ph_:
                    pt = [sb(ph_, f"pt{i}", [128, c.INW], BF16) for i in range(2)]
                    gx = sb(ph_, "gx", [128, 2 * c.A], F32)
                    t1 = sb(ph_, "t1", [128, 2 * c.A], F32)
                    vn = sb(ph_, "vn", [128, c.A], BF16)
                    ya = sb(ph_, "ya", [128, c.A], F32)
                    sgb = sb(ph_, "sgb", [128, c.A], F32)
                    wsT = sb(ph_, "wsT", [128, c.AG * 128], BF16)
                    bsT = sb(ph_, "bsT", [128, c.AG], F32)
                    gq = sb(ph_, "gq", [128, 128], F32); gk = sb(ph_, "gk", [128, 128], F32)
                    NH = c.CH + c.CKV
                    cx = sb(ph_, "cx", [128, NH * 128], F32)
                    cs = sb(ph_, "cs", [128, NH * 128], F32)
                    cr = sb(ph_, "cr", [128, NH * 128], F32)
                    cb = sb(ph_, "cb", [128, NH * 128], BF16)
                    qs = sb(ph_, "qs", [128, 2 * c.B], BF16)
                    rp = sb(ph_, "rp", [128, 128], F32)
                    tst = [sb(ph_, f"tst{i}", [128, 1024], BF16) for i in range(2)]
                    tr.dma("sp", sgb[:], sgu_gb[l * 128:(l + 1) * 128, :], w=["sgb"])
                    tr.dma("pool", wsT[:, :].rearrange("p (g q) -> p g q", q=128),
                           sgu_wT[l * c.AG * 128:(l + 1) * c.AG * 128, :].rearrange("(g p) q -> p g q", p=128), w=["wsT"])
                    tr.dma("sp", bsT[:], sgu_bT[l * 128:(l + 1) * 128, :], w=["bsT"])
                    tr.dma("sp", gq[:], qkg[(l * 2) * 128:(l * 2 + 1) * 128, :], w=["gq"])
                    tr.dma("sp", gk[:], qkg[(l * 2 + 1) * 128:(l * 2 + 2) * 128, :], w=["gk"])
                    k_ = dict(t=0)

                    def transposes_to(srcs, dstT, t, srcnames):
                        for h0 in range(0, len(srcs), 8):
                            hs = srcs[h0:h0 + 8]
                            pi = next_psb()
                            for i, a in enumerate(hs):
                                tr.op("pe", lambda g: g.transpose(psb[pi][:, i * 128:(i + 1) * 128], a, ident[:]), r=srcnames + ["ident"], w=[f"psb{pi}"])
                            ti = k_["t"] % 2; k_["t"] += 1
                            copy_op(ev_eng(), tst[ti][:, 0:len(hs) * 128], psb[pi][:, 0:len(hs) * 128], [f"psb{pi}"], [f"tst{ti}"])
                            tr.dma("sp", dstT[h0 * 128:(h0 + len(hs)) * 128, t * 128:(t + 1) * 128].rearrange("(h p) q -> p h q", p=128),
                                   tst[ti][:, 0:len(hs) * 128].rearrange("p (h q) -> p h q", q=128), r=[f"tst{ti}"], w=[f"T{t}"])

                    for t in range(c.TT):
                        isctx = t >= c.NT
                        pi_ = t % 2
                        P = pt[pi_]
                        pn = f"pt{pi_}"
                        tr.dma("sp", P[:], proj[t * 128:(t + 1) * 128, :], r=[f"proj{t}"], w=[pn])
                        X = P[:, c.o_au:c.o_au + 2 * c.A]
                        tr.op("dve", lambda g: g.tensor_tensor(t1[:], X, X, ALU.mult), r=[pn], w=["t1"])
                        tr.op("dve", lambda g: g.tensor_scalar(t1[:], t1[:], 0.044715, 1.0, ALU.mult, ALU.add), r=["t1"], w=["t1"])
                        tr.op("dve", lambda g: g.tensor_tensor(t1[:], t1[:], X, ALU.mult), r=["t1", pn], w=["t1"])
                        tr.op("act", lambda g: g.activation(out=t1[:], in_=t1[:], func=AF.Sigmoid, scale=1.5957691216057308), r=["t1"], w=["t1"])
                        tr.op("dve", lambda g: g.tensor_tensor(gx[:], t1[:], X, ALU.mult), r=["t1", pn], w=["gx"])
                        tr.op("act", lambda g: g.activation(out=t1[:, 0:c.A], in_=gx[:, c.A:2 * c.A], func=AF.Square, accum_out=stat[:, 8:9]), r=["gx"], w=["t1", "st8"])
                        rstd_from_ss(8, c.A, "st8")
                        tr.op("dve", lambda g: g.scalar_tensor_tensor(out=vn[:], in0=gx[:, c.A:2 * c.A], scalar=stat[:, 8:9], in1=sgb[:], op0=ALU.mult, op1=ALU.mult), r=["gx", "st8", "sgb"], w=["vn"])
                        wv = wsT[:, :].rearrange("p (g q) -> p g q", q=128)
                        banks = []
                        for g0 in range(0, c.AG, 4):
                            pi = next_psf(); banks.append(pi)
                            for gg in range(g0, min(c.AG, g0 + 4)):
                                tr.op("pe", lambda g: g.matmul(psf[pi][:, (gg - g0) * 128:(gg - g0 + 1) * 128], wv[:, gg, :], vn[:, gg * 128:(gg + 1) * 128], start=True, stop=True),
                                      r=["wsT", "vn"], w=[f"psf{pi}"])
                        for gg in range(c.AG):
                            pi = banks[gg // 4]
                            tr.op("dve", lambda g: g.scalar_tensor_tensor(out=ya[:, gg * 128:(gg + 1) * 128], in0=psf[pi][:, (gg % 4) * 128:(gg % 4 + 1) * 128], scalar=bsT[:, gg:gg + 1],
                                                                         in1=gx[:, gg * 128:(gg + 1) * 128], op0=ALU.add, op1=ALU.mult), r=[f"psf{pi}", "bsT", "gx"], w=["ya"])
                        tr.dma("sp", ysc[t * 128:(t + 1) * 128, 0:c.A], ya[:], r=["ya"], w=[f"ysc{t}a"])
                        tr.op("dve", lambda g: g.tensor_single_scalar(qs[:, 0:c.B], P[:, c.o_bq:c.o_bq + c.B], 128 ** -0.5, ALU.mult), r=[pn], w=["qs"])
                        transposes_to([qs[:, h * 128:(h + 1) * 128] for h in range(c.BH)], qbT, t, ["qs"])
                        transposes_to([P[:, c.o_bk + h * 128:c.o_bk + (h + 1) * 128] for h in range(c.BH)], kbT, t, [pn])
                        XC = P[:, c.o_cq:c.o_cq + NH * 128]
                        tr.op("dve", lambda g: g.tensor_tensor(cs[:], XC, XC, ALU.mult), r=[pn], w=["cs"])
                        tr.op("dve", lambda g: g.tensor_reduce(stat[:, 16:16 + NH], cs[:, :].rearrange("p (h d) -> p h d", d=128), AX.X, ALU.add), r=["cs"], w=["st16"])
                        tr.op("act", lambda g: g.activation(out=stat[:, 16:16 + NH], in_=stat[:, 16:16 + NH], func=AF.Sqrt, scale=1.0 / 128, bias=EPS), r=["st16"], w=["st16"])
                        tr.op("dve", lambda g: g.reciprocal(stat[:, 16:16 + NH], stat[:, 16:16 + NH]), r=["st16"], w=["st16"])
                        tr.op("dve", lambda g: g.tensor_single_scalar(stat[:, 16:16 + c.CH], stat[:, 16:16 + c.CH], 128 ** -0.5, ALU.mult), r=["st16"], w=["st16"])
                        c3 = lambda tile_: tile_[:, :].rearrange("p (h d) -> p h d", d=128)
                        tr.op("dve", lambda g: g.tensor_tensor(c3(cx), XC.rearrange("p (h d) -> p h d", d=128), stat[:, 16:16 + NH].unsqueeze(2).broadcast_to([128, NH, 128]), ALU.mult), r=[pn, "st16"], w=["cx"])
                        tr.op("dve", lambda g: g.tensor_tensor(c3(cx)[:, 0:c.CH, :], c3(cx)[:, 0:c.CH, :], gq[:, :].unsqueeze(1).broadcast_to([128, c.CH, 128]), ALU.mult), r=["cx", "gq"], w=["cx"])
                        tr.op("dve", lambda g: g.tensor_tensor(c3(cx)[:, c.CH:NH, :], c3(cx)[:, c.CH:NH, :], gk[:, :].unsqueeze(1).broadcast_to([128, c.CKV, 128]), ALU.mult), r=["cx", "gk"], w=["cx"])
                        if not isctx:
                            tr.dma("sp", rp[:], rope[t * 128:(t + 1) * 128, :], w=["rp"])
                            x5 = cx[:, :].rearrange("p (h a f) -> p h a f", a=4, f=32)
                            o5 = cr[:, :].rearrange("p (h a f) -> p h a f", a=4, f=32)
                            s5 = cs[:, :].rearrange("p (h a f) -> p h a f", a=4, f=32)
                            for ax in range(2):
                                cosb = rp[:, ax * 32:(ax + 1) * 32].unsqueeze(1).broadcast_to([128, NH, 32])
                                sinb = rp[:, 64 + ax * 32:64 + (ax + 1) * 32].unsqueeze(1).broadcast_to([128, NH, 32])
                                a_, b_ = x5[:, :, 2 * ax, :], x5[:, :, 2 * ax + 1, :]
                                tr.op("dve", lambda g: g.tensor_tensor(o5[:, :, 2 * ax, :], a_, cosb, ALU.mult), r=["cx", "rp"], w=["cr"])
                                tr.op("dve", lambda g: g.tensor_tensor(s5[:, :, 2 * ax, :], b_, sinb, ALU.mult), r=["cx", "rp"], w=["cs"])
                                tr.op("dve", lambda g: g.tensor_tensor(o5[:, :, 2 * ax, :], o5[:, :, 2 * ax, :], s5[:, :, 2 * ax, :], ALU.subtract), r=["cr", "cs"], w=["cr"])
                                tr.op("dve", lambda g: g.tensor_tensor(o5[:, :, 2 * ax + 1, :], a_, sinb, ALU.mult), r=["cx", "rp"], w=["cr"])
                                tr.op("dve", lambda g: g.tensor_tensor(s5[:, :, 2 * ax + 1, :], b_, cosb, ALU.mult), r=["cx", "rp"], w=["cs"])
                                tr.op("dve", lambda g: g.tensor_tensor(o5[:, :, 2 * ax + 1, :], o5[:, :, 2 * ax + 1, :], s5[:, :, 2 * ax + 1, :], ALU.add), r=["cr", "cs"], w=["cr"])
                            tr.op("act", lambda g: g.activation(out=cb[:], in_=cr[:], func=AF.Copy), r=["cr"], w=["cb"])
                        else:
                            tr.op("act", lambda g: g.activation(out=cb[:], in_=cx[:], func=AF.Copy), r=["cx"], w=["cb"])
                        transposes_to([cb[:, h * 128:(h + 1) * 128] for h in range(c.CH)], qcT, t, ["cb"])
                        transposes_to([cb[:, (c.CH + h) * 128:(c.CH + h + 1) * 128] for h in range(c.CKV)], kcT, t, ["cb"])
                    tr.barrier()

                with ExitStack() as ph_:
                    NKmax = c.T
                    KTs = sb(ph_, "KTs", [128, NKmax], BF16)
                    Vs = sb(ph_, "Vs", [128, c.TT * 128], BF16)
                    S = [sb(ph_, f"S{i}", [128, NKmax], F32) for i in range(2)]
                    Pm = [sb(ph_, f"Pm{i}", [128, NKmax], BF16) for i in range(2)]
                    PT = [sb(ph_, f"PT{i}", [128, 1024], BF16) for i in range(2)]
                    qt = [sb(ph_, f"qt{i}", [128, 128], BF16) for i in range(2)]
                    bia = [sb(ph_, f"bia{i}", [128, 1024], BF16) for i in range(2)]
                    yo = [sb(ph_, f"yo{i}", [128, 128], F32) for i in range(2)]
                    k_ = dict(i=0, pt=0)

                    def attn_jobs(jobs, ktname, vname):
                        def stage1(jb):
                            i = k_["i"] % 2; k_["i"] += 1
                            jb["slot"] = i
                            tr.dma("sp", qt[i][:], jb["qsrc"], r=[jb["qres"]], w=[f"qt{i}"])
                            if jb["bias"] is not None:
                                tr.dma("pool", bia[i][:], jb["bias"], w=[f"bia{i}"])
                            nk = jb["nk"]
                            pos = 0
                            for (k0, w) in jb["chunks"]:
                                pi = next_psf()
                                hasb = jb["bias"] is not None and pos < 1024
                                tr.op("pe", lambda g: g.matmul(psf[pi][:, 0:w], qt[i][:], KTs[:, k0:k0 + w], start=True, stop=not hasb), r=[f"qt{i}", ktname], w=[f"psf{pi}"])
                                if hasb:
                                    tr.op("pe", lambda g: g.matmul(psf[pi][:, 0:w], ident[:], bia[i][:, pos:pos + w], start=False, stop=True), r=[f"bia{i}", "ident"], w=[f"psf{pi}"])
                                copy_op(ev_eng(), S[i][:, pos:pos + w], psf[pi][:, 0:w], [f"psf{pi}"], [f"S{i}"])
                                pos += w
                            sc = 32 + 4 * i
                            tr.op("dve", lambda g: g.reduce_max(stat[:, sc:sc + 1], S[i][:, 0:nk], AX.X), r=[f"S{i}"], w=[f"sa{i}"])
                            tr.op("dve", lambda g: g.tensor_single_scalar(stat[:, sc:sc + 1], stat[:, sc:sc + 1], -1.0, ALU.mult), r=[f"sa{i}"], w=[f"sa{i}"])
                            tr.op("act", lambda g: g.activation(out=Pm[i][:, 0:nk], in_=S[i][:, 0:nk], func=AF.Exp, bias=stat[:, sc:sc + 1], scale=1.0, accum_out=stat[:, sc + 1:sc + 2]), r=[f"S{i}", f"sa{i}"], w=[f"Pm{i}", f"sb{i}"])
                            tr.op("dve", lambda g: g.reciprocal(stat[:, sc + 2:sc + 3], stat[:, sc + 1:sc + 2]), r=[f"sb{i}"], w=[f"sc{i}"])

                        def stage2(jb):
                            i = jb["slot"]
                            nkt = jb["nk"] // 128
                            po = next_psf()
                            for k0 in range(0, nkt, 8):
                                ks = list(range(k0, min(nkt, k0 + 8)))
                                pi = next_psb()
                                for ii, kt in enumerate(ks):
                                    tr.op("pe", lambda g: g.transpose(psb[pi][:, ii * 128:(ii + 1) * 128], Pm[i][:, kt * 128:(kt + 1) * 128], ident[:]), r=[f"Pm{i}", "ident"], w=[f"psb{pi}"])
                                ti = k_["pt"] % 2; k_["pt"] += 1
                                copy_op(ev_eng(), PT[ti][:, 0:len(ks) * 128], psb[pi][:, 0:len(ks) * 128], [f"psb{pi}"], [f"PT{ti}"])
                                for ii, kt in enumerate(ks):
                                    vt = jb["vt"][kt]
                                    tr.op("pe", lambda g: g.matmul(psf[po][:, 0:128], PT[ti][:, ii * 128:(ii + 1) * 128], Vs[:, vt * 128:(vt + 1) * 128], start=(kt == 0), stop=(kt == nkt - 1)),
                                          r=[f"PT{ti}", vname], w=[f"psf{po}"])
                            sc = 32 + 4 * i
                            tr.op("act", lambda g: g.activation(out=yo[i][:], in_=psf[po][:, 0:128], func=AF.Identity, scale=stat[:, sc + 2:sc + 3]), r=[f"psf{po}", f"sc{i}"], w=[f"yo{i}"])
                            r0, c0 = jb["dst"]
                            tr.dma("sp", ysc[r0:r0 + 128, c0:c0 + 128], yo[i][:], r=[f"yo{i}"], w=[f"ysc{r0 // 128}h{c0}"])

                        prev = None
                        for jb in jobs:
                            stage1(jb)
                            if prev is not None:
                                stage2(prev)
                            prev = jb
                        if prev is not None:
                            stage2(prev)

                    def chunks_of(segs):
                        out_ = []
                        for (k0, w) in segs:
                            for a in range(0, w, 512):
                                out_.append((k0 + a, min(512, w - a)))
                        return out_

                    qtiles = list(range(c.NT)) + (list(range(c.NT, c.TT)) if need_ctx else [])
                    for h in range(c.BH):
                        tr.dma("sp", KTs[:, :], kbT[h * 128:(h + 1) * 128, :], r=[f"T{t}" for t in range(c.TT)], w=["KTs"])
                        tr.dma("sp", Vs[:, :].rearrange("p (t d) -> p t d", d=128),
                               proj[:, c.o_bv + h * 128:c.o_bv + (h + 1) * 128].rearrange("(t p) d -> p t d", p=128), r=[f"proj{t}" for t in range(c.TT)], w=["Vs"])
                        jobs = []
                        for t in qtiles:
                            if t < c.NT:
                                s0 = tile_s0[t]
                                segs = [(s0 * 128, 1024), (c.N, c.M)]
                                vt = list(range(s0, s0 + 8)) + list(range(c.NT, c.TT))
                                kd = tile_kind[t]
                                r0 = ((l * NK + kd) * c.BH + h) * 128
                                bias = nab[r0:r0 + 128, :]
                            else:
                                segs = [(c.N, c.M)]; vt = list(range(c.NT, c.TT)); bias = None
                            jobs.append(dict(qsrc=qbT[h * 128:(h + 1) * 128, t * 128:(t + 1) * 128], qres=f"T{t}", chunks=chunks_of(segs), nk=sum(w for _, w in segs), vt=vt, bias=bias,
                                             dst=(t * 128, c.A + h * 128)))
                        attn_jobs(jobs, "KTs", "Vs")
                    gq2 = sb(ph_, "gq2", [128, 128], F32); gk2 = sb(ph_, "gk2", [128, 128], F32)
                    ones_b = sb(ph_, "ones_b", [128, 128], BF16)
                    qblk = [sb(ph_, f"qblk{i}", [128, 512], BF16) for i in range(2)]
                    PTs = [sb(ph_, f"PTs{i}", [128, 512], BF16) for i in range(3)]
                    rsb = sb(ph_, "rsb", [128, 512], F32)
                    OTs = sb(ph_, "OTs", [128, 512], F32)
                    yo4 = [sb(ph_, f"yo4{i}", [128, 512], F32) for i in range(2)]
                    tr.dma("sp", gq2[:], qkg[(l * 2) * 128:(l * 2 + 1) * 128, :], w=["gq2"])
                    tr.dma("sp", gk2[:], qkg[(l * 2 + 1) * 128:(l * 2 + 2) * 128, :], w=["gk2"])
                    tr.op("dve", lambda g: g.memset(ones_b[:], 1.0), w=["ones_b"])
                    tr.op("dve", lambda g: g.tensor_tensor(gq2[:], gq2[:], gq2[:], ALU.mult), r=["gq2"], w=["gq2"])
                    tr.op("dve", lambda g: g.tensor_tensor(gk2[:], gk2[:], gk2[:], ALU.mult), r=["gk2"], w=["gk2"])
                    tr.op("dve", lambda g: g.reduce_max(stat[:, 56:57], gq2[:], AX.X), r=["gq2"], w=["st56"])
                    tr.op("dve", lambda g: g.reduce_max(stat[:, 57:58], gk2[:], AX.X), r=["gk2"], w=["st57"])
                    tr.op("dve", lambda g: g.scalar_tensor_tensor(out=stat[:, 58:59], in0=stat[:, 56:57], scalar=128.0, in1=stat[:, 57:58], op0=ALU.mult, op1=ALU.mult), r=["st56", "st57"], w=["negC"])
                    tr.op("act", lambda g: g.activation(out=stat[:, 58:59], in_=stat[:, 58:59], func=AF.Sqrt), r=["negC"], w=["negC"])
                    tr.op("dve", lambda g: g.tensor_single_scalar(stat[:, 58:59], stat[:, 58:59], -1.0, ALU.mult), r=["negC"], w=["negC"])
                    negC = stat[:, 58:59]
                    jn = dict(n=0)

                    def gqa_job(h, q0, Wq, kts):
                        ji = jn["n"] % 2; jn["n"] += 1
                        pO, pS = 2 + 2 * ji, 3 + 2 * ji
                        tr.dma("sp", qblk[ji][:, 0:Wq], qcT[h * 128:(h + 1) * 128, q0:q0 + Wq], w=[f"qblk{ji}"])
                        nk = len(kts)

                        def qk(idx):
                            pb = idx % 2
                            tr.op("pe", lambda g: g.matmul(psf[pb][:, 0:Wq], KTs[:, kts[idx] * 128:(kts[idx] + 1) * 128], qblk[ji][:, 0:Wq], start=True, stop=True), r=["KTs", f"qblk{ji}"], w=[f"psf{pb}"])
                        qk(0)
                        for idx in range(nk):
                            if idx + 1 < nk:
                                qk(idx + 1)
                            pb = idx % 2; pt = idx % 3
                            tr.op("act", lambda g: g.activation(out=PTs[pt][:, 0:Wq], in_=psf[pb][:, 0:Wq], func=AF.Exp, bias=negC, scale=1.0), r=[f"psf{pb}", "negC"], w=[f"PTs{pt}"])
                            tr.op("pe", lambda g: g.matmul(psf[pO][:, 0:Wq], Vs[:, kts[idx] * 128:(kts[idx] + 1) * 128], PTs[pt][:, 0:Wq], start=(idx == 0), stop=(idx == nk - 1)), r=["Vs", f"PTs{pt}"], w=[f"psf{pO}"])
                            tr.op("pe", lambda g: g.matmul(psf[pS][:, 0:Wq], ones_b[:], PTs[pt][:, 0:Wq], start=(idx == 0), stop=(idx == nk - 1)), r=["ones_b", f"PTs{pt}"], w=[f"psf{pS}"])
                        tr.op("dve", lambda g: g.reciprocal(rsb[:, 0:Wq], psf[pS][:, 0:Wq]), r=[f"psf{pS}"], w=["rsb"])
                        tr.op("dve", lambda g: g.tensor_tensor(OTs[:, 0:Wq], psf[pO][:, 0:Wq], rsb[:, 0:Wq], ALU.mult), r=[f"psf{pO}", "rsb"], w=["OTs"])
                        pb = nk % 2
                        for i in range(Wq // 128):
                            tr.op("pe", lambda g: g.transpose(psf[pb][:, i * 128:(i + 1) * 128], OTs[:, i * 128:(i + 1) * 128], identf[:]), r=["OTs", "identf"], w=[f"psf{pb}"])
                        copy_op(ev_eng(), yo4[ji][:, 0:Wq], psf[pb][:, 0:Wq], [f"psf{pb}"], [f"yo4{ji}"])
                        col0 = c.A + c.B + h * 128
                        tr.dma("sp", ysc[q0:q0 + Wq, col0:col0 + 128].rearrange("(i p) d -> p i d", p=128), yo4[ji][:, 0:Wq].rearrange("p (i d) -> p i d", d=128), r=[f"yo4{ji}"], w=[f"yscC{q0}_{h}"])

                    for kv in range(c.CKV):
                        tr.dma("sp", KTs[:, :], kcT[kv * 128:(kv + 1) * 128, :], w=["KTs"])
                        tr.dma("sp", Vs[:, :].rearrange("p (t d) -> p t d", d=128),
                               proj[:, c.o_cv + kv * 128:c.o_cv + (kv + 1) * 128].rearrange("(t p) d -> p t d", p=128), w=["Vs"])
                        for q0 in range(0, c.N, 512):
                            for hh in range(4):
                                gqa_job(kv * 4 + hh, q0, min(512, c.N - q0), list(range(c.TT)))
                        if need_ctx:
                            for hh in range(4):
                                gqa_job(kv * 4 + hh, c.N, c.M, list(range(c.NT, c.TT)))
                    tr.barrier()

                moe = mf[l]
                j_ffn = sum(1 for m_ in mf[:l] if m_ == moe)
                if moe:
                    blocks = [(W["m1"][e * D:(e + 1) * D, :], W["m3"][e * D:(e + 1) * D, :], W["m2"][e * c.FFE:(e + 1) * c.FFE, :], c.FFE, e) for e in range(c.NE)]
                else:
                    FB = min(c.FF, 1024)
                    blocks = [(W["f1"][:, f0:f0 + FB], W["f3"][:, f0:f0 + FB], W["f2"][f0:f0 + FB, :], FB, None) for f0 in range(0, c.FF, FB)]
                FBmax = max(bk[3] for bk in blocks)
                with ExitStack() as ph_:
                    alloc_lin(ph_)
                    xg = sb(ph_, "xg", [128, G * D], F32)
                    xsa = sb(ph_, "xsa", [128, max(D, (FBmax // 128) * G * 128)], BF16)
                    xn = xsa[:, 0:D]
                    sa = xsa
                    ph = dict(xn=xn)
                    gb = sb(ph_, "gb", [128, D], BF16)
                    dg = sb(ph_, "dg", [128, 128], F32)
                    gT = sb(ph_, "gT", [128, (FBmax // 128) * G * 128], BF16)
                    gate = sb(ph_, "gate", [128, G * 8], F32)
                    lg = sb(ph_, "lg", [128, G * 8], F32)
                    mx8 = sb(ph_, "mx8", [128, 8], F32)
                    rw = sb(ph_, "rw", [128, KC * c.NE], BF16)
                    rbb = sb(ph_, "rbb", [128, c.NE], F32)
                    tmpy = [sb(ph_, f"tmpy{i}", [128, 512], F32) for i in range(2)]
                    if moe:
                        tr.dma("pool", rw[:, :].rearrange("p (k e) -> p k e", e=c.NE), moe_r[j_ffn * D:(j_ffn + 1) * D, :].rearrange("(k p) e -> p k e", p=128), w=["rw"])
                        tr.dma("sp", rbb[:], moe_rb[j_ffn * 128:(j_ffn + 1) * 128, :], w=["rbb"])
                    xgv = xg[:, :].rearrange("p (j d) -> p j d", d=D)
                    k_ = dict(y=0)

                    def make_gate_bcast(m, r_):
                        mvv = mod_ap(m, r_)
                        for kc in range(KC):
                            tr.op("dve", lambda g: g.tensor_single_scalar(dg[:], identf[:], mvv[:, kc:kc + 1], ALU.mult), r=["identf", "modT"], w=["dg"])
                            pi = next_psf()
                            tr.op("pe", lambda g: g.matmul(psf[pi][:, 0:128], ones_f[:], dg[:], start=True, stop=True), r=["ones_f", "dg"], w=[f"psf{pi}"])
                            copy_op("act", gb[:, kc * 128:(kc + 1) * 128], psf[pi][:, 0:128], [f"psf{pi}"], ["gb"])

                    for (t0, nt, isctx) in groups:
                        if isctx and not need_ctx:
                            continue
                        r_ = crow(isctx)
                        for j in range(nt):
                            rr = (t0 + j) * 128
                            tr.dma("sp", xgv[:, j, :], ysc[rr:rr + 128, :], w=[f"xg{j}"])
                            make_actT(ph, xgv[:, j, :], f"xg{j}", j, [(0, c.A), (c.A, c.A + c.B), (c.A + c.B, D)], gT_g, None, ["gT_g"])
                            tr.dma("sp", xgv[:, j, :], xsb[b][rr:rr + 128, :], r=[f"xs{b}_{t0 + j}"], w=[f"xg{j}"])
                        make_gate_bcast(2, r_)

                        def evac_res(j, c0, w, ps, psn):
                            yi = k_["y"] % 2; k_["y"] += 1
                            tr.op("dve", lambda g: g.tensor_tensor(tmpy[yi][:, 0:w], ps, gb[:, c0:c0 + w], ALU.mult), r=[psn, "gb"], w=[f"tmpy{yi}"])
                            tr.op("dve", lambda g: g.tensor_tensor(xgv[:, j, c0:c0 + w], xgv[:, j, c0:c0 + w], tmpy[yi][:, 0:w], ALU.add), r=[f"tmpy{yi}", f"xg{j}"], w=[f"xg{j}"])
                        linear(W["w_out"], D, D, nt, evac_res)
                        sv2 = s2T[:, :].rearrange("p (r k) -> p r k", k=KC)[:, r_, :]
                        for j in range(nt):
                            make_actT(ph, xgv[:, j, :], f"xg{j}", j, [(0, D)], sv2, mod_ap(3, r_), ["s2T", "modT"])
                        make_gate_bcast(5, r_)
                        ntok = nt * 128
                        av = cur["actT"][:, :].rearrange("p (k t) -> p k t", t=G * 128)
                        if moe:
                            rwv = rw[:, :].rearrange("p (k e) -> p k e", e=c.NE)
                            for j in range(nt):
                                pi = next_psf()
                                for kc in range(KC):
                                    tr.op("pe", lambda g: g.matmul(psf[pi][:, 0:c.NE], av[:, kc, j * 128:(j + 1) * 128], rwv[:, kc, :], start=(kc == 0), stop=(kc == KC - 1)), r=["actT", "rw"], w=[f"psf{pi}"])
                                lgj = lg[:, j * 8:(j + 1) * 8]; gj = gate[:, j * 8:(j + 1) * 8]
                                tr.op("dve", lambda g: g.tensor_tensor(lgj, psf[pi][:, 0:c.NE], rbb[:], ALU.add), r=[f"psf{pi}", "rbb"], w=["lg"])
                                tr.op("dve", lambda g: g.max(mx8[:], lgj), r=["lg"], w=["mx8"])
                                tr.op("dve", lambda g: g.tensor_single_scalar(gj, lgj, mx8[:, 1:2], ALU.is_ge), r=["lg", "mx8"], w=["gate"])
                                tr.op("dve", lambda g: g.tensor_single_scalar(mx8[:, 2:3], mx8[:, 0:1], -1.0, ALU.mult), r=["mx8"], w=["mx8"])
                                tr.op("act", lambda g: g.activation(out=lgj, in_=lgj, func=AF.Exp, bias=mx8[:, 2:3], scale=1.0), r=["lg", "mx8"], w=["lg"])
                                tr.op("dve", lambda g: g.tensor_tensor(gj, gj, lgj, ALU.mult), r=["gate", "lg"], w=["gate"])
                                tr.op("dve", lambda g: g.reduce_sum(mx8[:, 3:4], gj, AX.X), r=["gate"], w=["mx8"])
                                tr.op("dve", lambda g: g.reciprocal(mx8[:, 4:5], mx8[:, 3:4]), r=["mx8"], w=["mx8"])
                                tr.op("dve", lambda g: g.tensor_single_scalar(gj, gj, mx8[:, 4:5], ALU.mult), r=["gate", "mx8"], w=["gate"])
                        for (W1, W3, W2, FBk, e) in blocks:
                            sav = sa[:, 0:(FBk // 128) * ntok].rearrange("p (f t) -> p f t", t=ntok)
                            gv = gT[:, 0:(FBk // 128) * ntok].rearrange("p (f t) -> p f t", t=ntok)

                            def ev1(fc, ps, psn):
                                tr.op("act", lambda g: g.activation(out=sav[:, fc, :], in_=ps, func=AF.Silu), r=[psn], w=["xn"])

                            def ev3(fc, ps, psn):
                                tr.op("dve", lambda g: g.tensor_tensor(gv[:, fc, :], ps, sav[:, fc, :], ALU.mult), r=[psn, "xn"], w=["gT"])
                            linearT(W1, D, FBk, ntok, ev1)
                            linearT(W3, D, FBk, ntok, ev3)

                            def evac_ffn(j, c0, w, ps, psn):
                                yi = k_["y"] % 2; k_["y"] += 1
                                if e is None:
                                    tr.op("dve", lambda g: g.tensor_tensor(tmpy[yi][:, 0:w], ps, gb[:, c0:c0 + w], ALU.mult), r=[psn, "gb"], w=[f"tmpy{yi}"])
                                else:
                                    tr.op("dve", lambda g: g.scalar_tensor_tensor(out=tmpy[yi][:, 0:w], in0=ps, scalar=gate[:, j * 8 + e:j * 8 + e + 1], in1=gb[:, c0:c0 + w], op0=ALU.mult, op1=ALU.mult),
                                          r=[psn, "gb", "gate"], w=[f"tmpy{yi}"])
                                tr.op("dve", lambda g: g.tensor_tensor(xgv[:, j, c0:c0 + w], xgv[:, j, c0:c0 + w], tmpy[yi][:, 0:w], ALU.add), r=[f"tmpy{yi}", f"xg{j}"], w=[f"xg{j}"])
                            linear(W2, FBk, D, nt, evac_ffn, lhs_of=lambda kc, j: gv[:, kc, j * 128:(j + 1) * 128])
                        for j in range(nt):
                            rr = (t0 + j) * 128
                            tr.dma("sp", xsb[b][rr:rr + 128, :], xgv[:, j, :], r=[f"xg{j}"], w=[f"xs{b}_{t0 + j}"])
                    tr.barrier()
        if xout:
            for b in range(NB):
                for t in range(c.TT):
                    r0 = b * c.T + t * 128
                    tr.dma("sp", xo[r0:r0 + 128, :], xsb[b][t * 128:(t + 1) * 128, :], w=[f"xo{b}_{t}"])
        with ExitStack() as ph_:
          if final:
            finb = sb(ph_, "finb", [128, D], F32)
            xt = [sb(ph_, f"fx{i}", [128, D], F32) for i in range(2)]
            jk = sb(ph_, "fjk", [128, D], BF16)
            tr.dma("sp", finb[:], fin_b[:, :], w=["finb"])
            for b in range(NB):
                for t in range(c.NT):
                    i = t % 2
                    tr.dma("sp", xt[i][:], xsb[b][t * 128:(t + 1) * 128, :], w=[f"fx{i}"])
                    tr.op("act", lambda g: g.activation(out=jk[:], in_=xt[i][:], func=AF.Square, accum_out=stat[:, i:i + 1]), r=[f"fx{i}"], w=["fjk", f"fs{i}"])
                    rstd_from_ss(i, D, f"fs{i}")
                    tr.op("dve", lambda g: g.scalar_tensor_tensor(out=xt[i][:], in0=xt[i][:], scalar=stat[:, i:i + 1], in1=finb[:], op0=ALU.mult, op1=ALU.mult), r=[f"fx{i}", f"fs{i}", "finb"], w=[f"fx{i}"])
                    tr.dma("sp", out[b * c.N + t * 128:b * c.N + (t + 1) * 128, :], xt[i][:], r=[f"fx{i}"], w=[f"out{b}_{t}"])
        tr.final_wait()
    return nc


finb = None


def _build(cfg):
    global finb
    return build_program(cfg)


def prep_inputs(cfg, inp, core, xin_override=None):
    c = cfg
    f = lambda a: np.ascontiguousarray(np.asarray(a, dtype=np.float32))
    L, D, KC, NB = c.L, c.D, c.KC, c.NB
    bs = list(range(core * NB, (core + 1) * NB))
    if xin_override is not None:
        xin = xin_override
    else:
        x = f(inp["x"]); ctx = f(inp["ctx"])
        xin = np.concatenate([np.concatenate([x[b], ctx[b]], 0) for b in bs], 0)
    conds = np.stack([f(inp["c"])[b] for b in bs] + [f(inp["c_ctx"])], 0)
    cT = conds.T.reshape(KC, 128, c.R).transpose(1, 0, 2).reshape(128, KC * c.R)
    pT = lambda v: f(v).reshape(L, -1, 128).transpose(0, 2, 1)

    def rs(a, n):
        a = f(a)
        return a.reshape(-1, n) if (a.size >= n and a.size % n == 0) else np.zeros((1, n), np.float32)
    rpb = f(inp["na_rpb"])
    t = np.arange(c.N)
    pos = np.stack([t // c.GW, t % c.GW], -1).astype(np.float32)
    nf = c.HD // 4
    inv = (1.0 / (10000.0 ** (np.arange(nf, dtype=np.float32) / nf))).astype(np.float32)
    ang = (pos[:, :, None] * inv).astype(np.float32)
    rope = np.concatenate([np.cos(ang).reshape(c.N, 2 * nf), np.sin(ang).reshape(c.N, 2 * nf)], 1).astype(np.float32)
    d = dict(
        xin=xin, cT=f(cT),
        ada_down=f(inp["ada_down"]).reshape(L * D, c.RANK), ada_up=f(inp["ada_up"]).reshape(L * c.RANK, 6 * D),
        ada_biasT=f(pT(inp["ada_bias"]).reshape(L * 128, 6 * KC)),
        n1T=f(pT(inp["norm1_g"]).reshape(L * 128, KC)), n2T=f(pT(inp["norm2_g"]).reshape(L * 128, KC)), grpT=f(pT(inp["group_norm_g"]).reshape(L * 128, KC)),
        w_in=f(inp["w_in"]).reshape(L * D, c.INW), w_out=f(inp["w_out"]).reshape(L * D, D),
        sgu_gb=f(np.broadcast_to(f(inp["sgu_norm_g"])[:, None, :], (L, 128, c.A)).reshape(L * 128, c.A)),
        sgu_wT=f(f(inp["sgu_w"]).transpose(0, 1, 3, 2).reshape(L * c.AG * 128, 128)),
        sgu_bT=f(f(inp["sgu_b"]).transpose(0, 2, 1).reshape(L * 128, c.AG)),
        nab=f(host_nab(c, rpb).reshape(-1, 1024)),
        qkg=f(np.broadcast_to(f(inp["qk_norm_g"])[:, :, None, :], (L, 2, 128, 128)).reshape(L * 2 * 128, 128)),
        ffn_w1=rs(inp["ffn_w1"], c.FF), ffn_w3=rs(inp["ffn_w3"], c.FF), ffn_w2=rs(inp["ffn_w2"], D),
        moe_r=rs(inp["moe_router"], c.NE),
        moe_rb=f(np.broadcast_to(rs(inp["moe_router_b"], c.NE)[:, None, :], (rs(inp["moe_router_b"], c.NE).shape[0], 128, c.NE)).reshape(-1, c.NE)),
        moe_w1=rs(inp["moe_w1"], c.FFE), moe_w3=rs(inp["moe_w3"], c.FFE), moe_w2=rs(inp["moe_w2"], D),
        fin_b=f(np.broadcast_to(f(inp["final_norm_g"])[None, :], (128, D))),
        ident=np.eye(128, dtype=np.float32), rope=rope,
    )
    return d


def run(cfg, inputs):
    ncores = cfg.BATCH // cfg.NB
    nc = _build(cfg)
    in_maps = [prep_inputs(cfg, inputs, k) for k in range(ncores)]
    res = run_bass_kernel_spmd(nc, in_maps, core_ids=list(range(ncores)))
    outs = [res.results[k]["out"].reshape(cfg.NB, cfg.N, cfg.D) for k in range(ncores)]
    return np.concatenate(outs, 0).astype(np.float32)


def build_final_norm(rows, D):
    nc = bass.Bass("TRN2", target_bir_lowering=False)
    xf = nc.dram_tensor("xf", [rows, D], F32, kind="ExternalInput").ap()
    fin_b = nc.dram_tensor("fin_b", [128, D], F32, kind="ExternalInput").ap()
    out = nc.dram_tensor("out", [rows, D], F32, kind="ExternalOutput").ap()
    with ExitStack() as top:
        tr = Tracker(nc, top)
        sbt = lambda name, shape, dt: top.enter_context(nc.sbuf_tensor(name, list(shape), dt))
        finb = sbt("finb", [128, D], F32)
        xt = [sbt(f"fx{i}", [128, D], F32) for i in range(2)]
        jk = sbt("fjk", [128, D], BF16)
        stat = sbt("fstat", [128, 8], F32)
        tr.dma("sp", finb[:], fin_b[:, :], w=["finb"])
        for t in range(rows // 128):
            i = t % 2
            tr.dma("sp", xt[i][:], xf[t * 128:(t + 1) * 128, :], w=[f"fx{i}"])
            tr.op("act", lambda g: g.activation(out=jk[:], in_=xt[i][:], func=AF.Square, accum_out=stat[:, i:i + 1]), r=[f"fx{i}"], w=["fjk", f"fs{i}"])
            tr.op("act", lambda g: g.activation(out=stat[:, i:i + 1], in_=stat[:, i:i + 1], func=AF.Sqrt, scale=1.0 / D, bias=EPS), r=[f"fs{i}"], w=[f"fs{i}"])
            tr.op("dve", lambda g: g.reciprocal(stat[:, i:i + 1], stat[:, i:i + 1]), r=[f"fs{i}"], w=[f"fs{i}"])
            tr.op("dve", lambda g: g.scalar_tensor_tensor(out=xt[i][:], in0=xt[i][:], scalar=stat[:, i:i + 1], in1=finb[:], op0=ALU.mult, op1=ALU.mult), r=[f"fx{i}", f"fs{i}", "finb"], w=[f"fx{i}"])
            tr.dma("sp", out[t * 128:(t + 1) * 128, :], xt[i][:], r=[f"fx{i}"], w=[f"out{t}"])
        tr.final_wait()
    return nc


_PER_LAYER = ("ada_down", "ada_up", "ada_bias", "norm1_g", "norm2_g", "w_in", "sgu_norm_g", "sgu_w", "sgu_b", "na_rpb", "qk_norm_g", "group_norm_g", "w_out")
_DENSE = ("ffn_w1", "ffn_w3", "ffn_w2")
_MOE = ("moe_router", "moe_router_b", "moe_w1", "moe_w3", "moe_w2")


def run_layers(cfg_full, inputs, NB=1):
    cf = cfg_full
    ncores = cf.BATCH // NB
    cfgL = Cfg(D=cf.D, BATCH=cf.BATCH, SEQ=cf.N, DEPTH=1, CTX=cf.M, RANK=cf.RANK, NB=NB)
    progs = {}
    xcur = [None] * ncores
    for l in range(cf.L):
        moe = (l % 2 == 1)
        if moe not in progs:
            progs[moe] = build_program(cfgL, moe_flags=[moe], need_ctx_flag=True, final=False, xout=True)
        inp_l = {}
        for k, v in inputs.items():
            if k in _PER_LAYER:
                inp_l[k] = np.asarray(v)[l:l + 1]
            elif k in _DENSE:
                if not moe:
                    inp_l[k] = np.asarray(v)[l // 2:l // 2 + 1]
            elif k in _MOE:
                if moe:
                    inp_l[k] = np.asarray(v)[l // 2:l // 2 + 1]
            else:
                inp_l[k] = v
        for k in (_MOE if not moe else _DENSE):
            inp_l[k] = np.zeros((1, 1), np.float32)
        in_maps = []
        for k in range(ncores):
            d = prep_inputs(cfgL, inp_l, k, xin_override=xcur[k])
            in_maps.append(d)
        res = run_bass_kernel_spmd(progs[moe], in_maps, core_ids=list(range(ncores)))
        xcur = [np.asarray(res.results[k]["xout"]) for k in range(ncores)]
        del in_maps, res
    lat = np.concatenate([xcur[k].reshape(NB, cf.T, cf.D)[:, :cf.N, :] for k in range(ncores)], 0).reshape(cf.BATCH * cf.N, cf.D)
    n8 = 8
    rows = lat.shape[0] // n8
    ncf = build_final_norm(rows, cf.D)
    finb = np.ascontiguousarray(np.broadcast_to(np.asarray(inputs["final_norm_g"], np.float32)[None, :], (128, cf.D)))
    res = run_bass_kernel_spmd(ncf, [{"xf": np.ascontiguousarray(lat[k * rows:(k + 1) * rows]), "fin_b": finb} for k in range(n8)], core_ids=list(range(n8)))
    out = np.concatenate([np.asarray(res.results[k]["out"]) for k in range(n8)], 0)
    return out.reshape(cf.BATCH, cf.N, cf.D).astype(np.float32)


def kernel(**inputs):
    cfg = Cfg(D=4096, BATCH=2, SEQ=8192, DEPTH=4, CTX=256, RANK=1024, NB=1)
    return run_layers(cfg, inputs, NB=1)
```
